# Optimizing a Trainium2 kernel written in Bass

```python
import math
import jax, jax.numpy as jnp
from jax import lax
import numpy as np

D_MODEL = 1024
BATCH = 8
SEQ = 4096
DEPTH = 1

CHUNK = 64
N_META = 16
Q_BLOCK = 128
HEAD_DIM = 64
ROPE_DIM = HEAD_DIM // 4
ROPE_THETA = 500000.0
SB_HEADS = 8
DSA_HEADS = 8
KV_LATENT = 128
NOPE_DIM = HEAD_DIM - ROPE_DIM
IDX_HEADS = 8
IDX_DIM = 64
TOPK_MAX = 256
N_GROUPS = 4
EXP_PER_GROUP = 8
N_EXPERTS = N_GROUPS * EXP_PER_GROUP
TOP_E = 2
D_EXPERT = 512
EXPERT_BLOCK = 128
NORM_EPS = 1e-6
SB_W = SB_HEADS * HEAD_DIM
DSA_W = DSA_HEADS * HEAD_DIM
SPLITS = (SB_W, SB_W, SB_W, DSA_W, KV_LATENT, ROPE_DIM, IDX_HEADS * IDX_DIM, IDX_DIM, IDX_HEADS, D_MODEL, D_MODEL)
IN_WIDTH = 3 * SB_W + DSA_W + KV_LATENT + ROPE_DIM + IDX_HEADS * IDX_DIM + IDX_DIM + IDX_HEADS + 2 * D_MODEL

kernel_name = "hybrid_sb_dsa_hmoe_meta"


def rmsnorm(x, g):
    xf = x.astype(jnp.float32)
    y = xf * lax.rsqrt(jnp.mean(xf * xf, axis=-1, keepdims=True) + NORM_EPS)
    return (y * g.astype(jnp.float32)).astype(x.dtype)


def partial_rope(x, pos):
    half = ROPE_DIM // 2
    inv = ROPE_THETA ** (-jnp.arange(half, dtype=jnp.float32) / half)
    ang = pos.astype(jnp.float32)[:, None] * inv[None, :]
    cos = jnp.cos(ang)[None, :, None, :]
    sin = jnp.sin(ang)[None, :, None, :]
    x1 = x[..., :half].astype(jnp.float32)
    x2 = x[..., half:ROPE_DIM].astype(jnp.float32)
    rot = jnp.concatenate([x1 * cos - x2 * sin, x2 * cos + x1 * sin], axis=-1).astype(x.dtype)
    return jnp.concatenate([rot, x[..., ROPE_DIM:]], axis=-1)


def chunk_ids(pos):
    return jnp.where(pos < N_META, 0, 1 + (pos - N_META) // CHUNK)


def to_blocks(a, t_pad):
    b, t = a.shape[0], a.shape[1]
    pad = [(0, 0), (0, t_pad - t)] + [(0, 0)] * (a.ndim - 2)
    a = jnp.pad(a, pad).reshape((b, t_pad // Q_BLOCK, Q_BLOCK) + a.shape[2:])
    return jnp.moveaxis(a, 1, 0)


def from_blocks(a, t):
    a = jnp.moveaxis(a, 0, 1)
    return a.reshape((a.shape[0], a.shape[1] * a.shape[2]) + a.shape[3:])[:, :t]


def stick_breaking(q, k, v, pos):
    t = q.shape[1]
    t_pad = -(-t // Q_BLOCK) * Q_BLOCK
    scale = 1.0 / math.sqrt(HEAD_DIM)
    qpos = jnp.arange(t_pad).reshape(t_pad // Q_BLOCK, Q_BLOCK)

    def block(args):
        qb, qp = args
        z = jnp.einsum('bqhd,bkhd->bhqk', qb, k).astype(jnp.float32) * scale
        strict = pos[None, :] < qp[:, None]
        log_keep = jnp.where(strict, jax.nn.log_sigmoid(-z), 0.0)
        later = lax.cumsum(log_keep, axis=3, reverse=True) - log_keep
        a = jnp.where(strict, jnp.exp(jax.nn.log_sigmoid(z) + later), 0.0)
        return jnp.einsum('bhqk,bkhd->bqhd', a.astype(v.dtype), v)

    out = lax.map(block, (to_blocks(q, t_pad), qpos))
    return from_blocks(out, t)


def dsa_attention(q_full, kv_all, q_idx, w_idx, k_idx, pos, topk):
    t = q_full.shape[1]
    t_pad = -(-t // Q_BLOCK) * Q_BLOCK
    scale = 1.0 / math.sqrt(HEAD_DIM)
    cid = chunk_ids(pos)
    qcid = chunk_ids(jnp.arange(t_pad)).reshape(t_pad // Q_BLOCK, Q_BLOCK)

    def block(args):
        qf, qi, wi, qc = args
        s_idx = jax.nn.relu(jnp.einsum('bqhd,bkd->bqhk', qi, k_idx))
        score = jnp.einsum('bqh,bqhk->bqk', wi, s_idx).astype(jnp.float32)
        adm = cid[None, :] <= qc[:, None]
        score = jnp.where(adm[None], score, -jnp.inf)
        _, sel = lax.top_k(score, topk)
        valid = cid[sel] <= qc[None, :, None]
        kv_sel = jax.vmap(lambda kv, i: kv[i])(kv_all, sel)
        logits = jnp.einsum('bqhc,bqkc->bqhk', qf, kv_sel).astype(jnp.float32) * scale
        logits = jnp.where(valid[:, :, None, :], logits, -1e30)
        p = jax.nn.softmax(logits, axis=-1)
        return jnp.einsum('bqhk,bqkc->bqhc', p.astype(kv_sel.dtype), kv_sel[..., ROPE_DIM:])

    out = lax.map(block, (to_blocks(q_full, t_pad), to_blocks(q_idx, t_pad), to_blocks(w_idx, t_pad), qcid))
    return from_blocks(out, t)


def mixer(u, w_in, w_uk, w_uv, w_up_a, w_up_b, w_o, pos, topk):
    b, t, _ = u.shape
    proj = u @ w_in
    qa, ka, va, qb, ckv, krope, qi, ki, wi, ga, gb = jnp.split(proj, list(np.cumsum(SPLITS)[:-1]), axis=-1)
    y_a = stick_breaking(qa.reshape(b, t, SB_HEADS, HEAD_DIM), ka.reshape(b, t, SB_HEADS, HEAD_DIM),
                         va.reshape(b, t, SB_HEADS, HEAD_DIM), pos).reshape(b, t, SB_W)
    qb = partial_rope(qb.reshape(b, t, DSA_HEADS, HEAD_DIM), pos)
    q_lat = jnp.einsum('bthn,hcn->bthc', qb[..., ROPE_DIM:], w_uk)
    q_full = jnp.concatenate([qb[..., :ROPE_DIM], q_lat], axis=-1)
    krope = partial_rope(krope[:, :, None, :], pos)[:, :, 0]
    kv_all = jnp.concatenate([krope, ckv], axis=-1)
    qi = partial_rope(qi.reshape(b, t, IDX_HEADS, IDX_DIM), pos)
    ki = partial_rope(ki[:, :, None, :], pos)[:, :, 0]
    o_lat = dsa_attention(q_full, kv_all, qi, wi, ki, pos, topk)
    y_b = jnp.einsum('bthc,hcd->bthd', o_lat, w_uv).reshape(b, t, DSA_W)
    z = jax.nn.sigmoid(ga) * (y_a @ w_up_a) + jax.nn.sigmoid(gb) * (y_b @ w_up_b)
    return z @ w_o


def hier_moe(u, w_group, b_group, w_router, b_router, w1, w3, w2):
    n, d = u.shape
    g_logits = (u @ w_group).astype(jnp.float32) + b_group.astype(jnp.float32)
    g_prob = jax.nn.softmax(g_logits, axis=-1)
    p_grp, g_sel = lax.top_k(g_prob, 1)
    e_logits = ((u @ w_router).astype(jnp.float32) + b_router.astype(jnp.float32)).reshape(n, N_GROUPS, EXP_PER_GROUP)
    gidx = jnp.broadcast_to(g_sel[:, :, None], (n, 1, EXP_PER_GROUP))
    in_grp = jnp.take_along_axis(e_logits, gidx, axis=1)[:, 0]
    e_prob = jax.nn.softmax(in_grp, axis=-1)
    p_exp, e_sel = lax.top_k(e_prob, TOP_E)
    gate = p_grp * p_exp / jnp.sum(p_exp, axis=-1, keepdims=True)
    expert = g_sel * EXP_PER_GROUP + e_sel
    m = n * TOP_E
    e_flat = expert.reshape(m).astype(jnp.int32)
    tok = jnp.repeat(jnp.arange(n, dtype=jnp.int32), TOP_E)
    w_flat = gate.reshape(m)
    order = jnp.argsort(e_flat)
    e_s = e_flat[order]
    counts = jnp.zeros((N_EXPERTS,), jnp.int32).at[e_flat].add(1)
    padded = (counts + EXPERT_BLOCK - 1) // EXPERT_BLOCK * EXPERT_BLOCK
    p_end = jnp.cumsum(padded)
    p_start = p_end - padded
    start = jnp.cumsum(counts) - counts
    dest = p_start[e_s] + jnp.arange(m, dtype=jnp.int32) - start[e_s]
    n_blk = -(-m // EXPERT_BLOCK) + N_EXPERTS
    p_tot = n_blk * EXPERT_BLOCK
    buf_tok = jnp.full((p_tot,), n, jnp.int32).at[dest].set(tok[order])
    buf_w = jnp.zeros((p_tot,), jnp.float32).at[dest].set(w_flat[order])
    blk_e = jnp.minimum(jnp.searchsorted(p_end, jnp.arange(n_blk, dtype=jnp.int32) * EXPERT_BLOCK, side='right'), N_EXPERTS - 1)
    u_pad = jnp.concatenate([u, jnp.zeros((1, d), u.dtype)], axis=0)
    xb = u_pad[buf_tok].reshape(n_blk, EXPERT_BLOCK, d)

    def expert_block(args):
        xe, e = args
        hid = jax.nn.silu(xe @ w1[e]) * (xe @ w3[e])
        return hid @ w2[e]

    yb = lax.map(expert_block, (xb, blk_e)).reshape(p_tot, d)
    out = jnp.zeros((n + 1, d), u.dtype).at[buf_tok].add(yb * buf_w[:, None].astype(yb.dtype))
    return out[:n]


def setup_inputs(seed: int = 0) -> dict:
    key = jax.random.key(seed)
    ks = jax.random.split(key, 20)
    f32 = jnp.float32
    nrm = lambda k, shape, s: jax.random.normal(k, shape, f32) * s
    return {
        "x": nrm(ks[0], (BATCH, SEQ, D_MODEL), 1.0),
        "meta_tokens": nrm(ks[1], (N_META, D_MODEL), 1.0),
        "norm_mix_g": 1.0 + nrm(ks[2], (DEPTH, D_MODEL), 0.02),
        "w_in": nrm(ks[3], (DEPTH, D_MODEL, IN_WIDTH), D_MODEL ** -0.5),
        "w_uk": nrm(ks[4], (DEPTH, DSA_HEADS, KV_LATENT, NOPE_DIM), KV_LATENT ** -0.5),
        "w_uv": nrm(ks[5], (DEPTH, DSA_HEADS, KV_LATENT, HEAD_DIM), KV_LATENT ** -0.5),
        "w_up_a": nrm(ks[6], (DEPTH, SB_W, D_MODEL), SB_W ** -0.5),
        "w_up_b": nrm(ks[7], (DEPTH, DSA_W, D_MODEL), DSA_W ** -0.5),
        "w_o": nrm(ks[8], (DEPTH, D_MODEL, D_MODEL), D_MODEL ** -0.5),
        "norm_ffn_g": 1.0 + nrm(ks[9], (DEPTH, D_MODEL), 0.02),
        "w_group": nrm(ks[10], (DEPTH, D_MODEL, N_GROUPS), D_MODEL ** -0.5),
        "b_group": nrm(ks[11], (DEPTH, N_GROUPS), 0.01),
        "w_router": nrm(ks[12], (DEPTH, D_MODEL, N_EXPERTS), D_MODEL ** -0.5),
        "b_router": nrm(ks[13], (DEPTH, N_EXPERTS), 0.01),
        "w1": nrm(ks[14], (DEPTH, N_EXPERTS, D_MODEL, D_EXPERT), D_MODEL ** -0.5),
        "w3": nrm(ks[15], (DEPTH, N_EXPERTS, D_MODEL, D_EXPERT), D_MODEL ** -0.5),
        "w2": nrm(ks[16], (DEPTH, N_EXPERTS, D_EXPERT, D_MODEL), D_EXPERT ** -0.5),
        "norm_final_g": 1.0 + nrm(ks[17], (D_MODEL,), 0.02),
    }


def reference(x, meta_tokens, norm_mix_g, w_in, w_uk, w_uv, w_up_a, w_up_b, w_o, norm_ffn_g,
              w_group, b_group, w_router, b_router, w1, w3, w2, norm_final_g):
    b, s, d = x.shape
    topk = min(TOPK_MAX, s // 4)
    meta = jnp.broadcast_to(meta_tokens[None].astype(x.dtype), (b, N_META, d))
    h = jnp.concatenate([meta, x], axis=1)
    t = s + N_META
    pos = jnp.arange(t)
    for l in range(DEPTH):
        h = h + mixer(rmsnorm(h, norm_mix_g[l]), w_in[l], w_uk[l], w_uv[l], w_up_a[l], w_up_b[l], w_o[l], pos, topk)
        u = rmsnorm(h, norm_ffn_g[l]).reshape(b * t, d)
        h = h + hier_moe(u, w_group[l], b_group[l], w_router[l], b_router[l], w1[l], w3[l], w2[l]).reshape(b, t, d)
    return rmsnorm(h, norm_final_g)[:, N_META:]
```

```python
import numpy as np
from contextlib import ExitStack
import concourse.bass as bass
import concourse.mybir as mybir
from concourse.bass_utils import run_bass_kernel_spmd

F32 = mybir.dt.float32
BF16 = mybir.dt.bfloat16
I32 = mybir.dt.int32
AF = mybir.ActivationFunctionType
ALU = mybir.AluOpType
AX = mybir.AxisListType

NCORES = 8
SEQ = 4096
NMETA = 16
T = 4224
NT = T // 128
D = 1024
EPS = 1e-6
QB = 384
NQB = T // QB
CAP = 384
NEXP = 32
NSLOT = NEXP * CAP
NITER = 16
TOPK = 256

O_QA, O_KA, O_VA, O_QB, O_CKV, O_KR, O_QI, O_KI, O_WI, O_GA, O_GB = (
    0, 512, 1024, 1536, 2048, 2176, 2192, 2704, 2768, 2776, 3800)

B_QBN, B_QBR, B_QBRT, B_KR, B_KRT, B_KI, B_KIT, B_QI0, B_QI0T, B_QI1, B_QI1T, B_CKV, B_WI, B_GA, B_GB = (
    0, 512, 768, 1024, 1152, 1280, 1408, 1536, 1792, 2048, 2304, 2560, 2688, 2696, 3720)
NB = 4744

C_ID, C_TN, C_ON, C_OP, C_TS, C_SBM, C_MD, C_MN, C_IO, C_P2 = (
    0, 128, 256, 384, 512, 640, 640 + 1152, 640 + 1152 + 128, 640 + 1152 + 256, 640 + 1152 + 256 + 32)
NCST = C_P2 + 2 * NITER + 2


class Op:
    __slots__ = ("eng", "fn", "dma", "deps", "signals", "sem", "val", "didx")

    def __init__(self, eng, fn, dma):
        self.eng = eng
        self.fn = fn
        self.dma = dma
        self.deps = []
        self.signals = False
        self.sem = None
        self.val = 0
        self.didx = -1


class Phase:
    K = 6
    ENGS = ("pe", "act", "dve", "pool", "sp")

    def __init__(self, nc, name):
        self.nc = nc
        self.name = name
        self.es = ExitStack()
        self.ops = {e: [] for e in self.ENGS}
        self.lastw = {}
        self.rd_c = {}
        self.rd_d = {}

    def __enter__(self):
        self.es.__enter__()
        return self

    def __exit__(self, *a):
        if a[0] is None:
            self.emit()
        return self.es.__exit__(*a)

    def sb(self, name, shape, dt):
        return self.es.enter_context(self.nc.sbuf_tensor(f"{self.name}_{name}", shape, dt))

    def ps(self, name, shape, dt=F32):
        return self.es.enter_context(self.nc.psum_tensor(f"{self.name}_{name}", shape, dt))

    def _dep(self, o, d, raw):
        if d is o:
            return
        if (not o.dma) and (not d.dma) and o.eng == d.eng:
            if not raw or o.eng == "pe":
                return
        if d not in o.deps:
            o.deps.append(d)
            d.signals = True

    def op(self, eng, fn, r=(), w=(), dma=False):
        o = Op(eng, fn, dma)
        for k in r:
            lw = self.lastw.get(k)
            if lw is not None:
                self._dep(o, lw, True)
        for k in w:
            lw = self.lastw.get(k)
            if lw is not None:
                self._dep(o, lw, False)
            for d in self.rd_c.get(k, {}).values():
                self._dep(o, d, False)
            for d in self.rd_d.get(k, ()):
                self._dep(o, d, False)
        for k in r:
            if dma:
                self.rd_d.setdefault(k, []).append(o)
            else:
                self.rd_c.setdefault(k, {})[eng] = o
        for k in w:
            self.lastw[k] = o
            self.rd_c[k] = {}
            self.rd_d[k] = []
        self.ops[eng].append(o)
        return o

    def dma(self, out, in_, r=(), w=(), eng="sp", **kw):
        return self.op(eng, lambda e: e.dma_start(out=out, in_=in_, **kw), r=r, w=w, dma=True)

    def emit(self):
        nc = self.nc
        es = self.es
        K = self.K
        dsems = {}
        dlist = {}
        for e in self.ENGS:
            c = 0
            dl = []
            csem = None
            for o in self.ops[e]:
                if o.dma:
                    n = len(dl)
                    if e not in dsems:
                        dsems[e] = [es.enter_context(nc.semaphore(f"{self.name}_d{e}{i}")) for i in range(K)]
                    o.didx = n
                    o.sem = (f"d{e}{n % K}", dsems[e][n % K])
                    o.val = 16 * (n // K + 1)
                    dl.append(o)
                elif o.signals:
                    if csem is None:
                        csem = es.enter_context(nc.semaphore(f"{self.name}_c{e}"))
                    c += 1
                    o.sem = (f"c{e}", csem)
                    o.val = c
            dlist[e] = dl
        hmap = {"pe": "tensor", "act": "scalar", "dve": "vector", "pool": "gpsimd", "sp": "sync"}
        with nc.Block() as block:
            for e in self.ENGS:
                if not self.ops[e]:
                    continue

                def body(eng, e=e):
                    waited = {}
                    for o in self.ops[e]:
                        waits = {}
                        for d in o.deps:
                            nm, s = d.sem
                            if waits.get(nm, (0, None))[0] < d.val:
                                waits[nm] = (d.val, s)
                        if o.dma and o.didx >= K:
                            p = dlist[e][o.didx - K]
                            nm, s = p.sem
                            if waits.get(nm, (0, None))[0] < p.val:
                                waits[nm] = (p.val, s)
                        for nm, (v, s) in waits.items():
                            if waited.get(nm, 0) < v:
                                eng.wait_ge(s, v)
                                waited[nm] = v
                        ins = o.fn(eng)
                        if o.dma:
                            ins.then_inc(o.sem[1], 16)
                        elif o.signals:
                            ins.then_inc(o.sem[1], 1)
                    fin = {}
                    for o in dlist[e]:
                        fin[o.sem[0]] = (o.val, o.sem[1])
                    for nm, (v, s) in fin.items():
                        if waited.get(nm, 0) < v:
                            eng.wait_ge(s, v)

                getattr(block, hmap[e])(body)


class Ring:
    def __init__(self, name, bufs):
        self.name = name
        self.bufs = bufs
        self.i = -1

    def next(self):
        self.i += 1
        j = self.i % len(self.bufs)
        return self.bufs[j], (self.name, j)


def build_program(debug_out=(), stop_after=99):
    nc = bass.Bass("TRN2", target_bir_lowering=False)

    def din(name, shape, dt=F32):
        return nc.dram_tensor(name, list(shape), dt, kind="ExternalInput")

    def dscr(name, shape, dt):
        kind = "ExternalOutput" if name in debug_out else "Internal"
        return nc.dram_tensor(name, list(shape), dt, kind=kind)

    h0 = din("h0", [T, D])
    g1 = din("g1", [1, D])
    g2 = din("g2", [1, D])
    gF = din("gF", [1, D])
    winA = din("winA", [D, 1536])
    winB = din("winB", [D, NB])
    tabs = din("tabs", [4, 128, T])
    cst = din("cst", [128, NCST])
    wukT = din("wukT", [128, 4, 128])
    wuv = din("wuv", [128, 8, 64])
    wupa = din("wupa", [512, D])
    wupb = din("wupb", [512, D])
    wo = din("wo", [D, D])
    wr = din("wr", [D, 36])
    br = din("br", [1, 36])
    if stop_after >= 5:
        w1 = din("w1", [NEXP, D, 512])
        w3 = din("w3", [NEXP, D, 512])
        w2 = din("w2", [NEXP, 512, D])
    out = nc.dram_tensor("out", [SEQ, D], F32, kind="ExternalOutput")

    unT_d = dscr("unT_d", [128, 8, T], BF16)
    QA_d = dscr("QA_d", [512, T], BF16)
    KA_d = dscr("KA_d", [512, T], BF16)
    VA_d = dscr("VA_d", [T, 512], BF16)
    QBN_d = dscr("QBN_d", [512, T], BF16)
    QBR_d = dscr("QBR_d", [256, T], BF16)
    KR_d = dscr("KR_d", [128, T], BF16)
    KI_d = dscr("KI_d", [128, T], BF16)
    QI_d = dscr("QI_d", [512, T], BF16)
    CKVT_d = dscr("CKVT_d", [128, T], BF16)
    CKV_d = dscr("CKV_d", [T, 128], BF16)
    WI_d = dscr("WI_d", [T, 8], F32)
    YA_d = dscr("YA_d", [512, T], BF16)
    YB_d = dscr("YB_d", [512, T], BF16)
    H1_d = dscr("H1_d", [T, D], F32)
    XS_d = dscr("XS_d", [NSLOT + 128, D], BF16)
    YS_d = dscr("YS_d", [NSLOT + 128, D], F32)

    blocks512 = [(i * 512, 512) for i in range(8)] + [(4096, 128)]
    outer = ExitStack()
    slots_all = outer.enter_context(nc.sbuf_tensor("slots_all", [128, NT, 2], I32))
    gates_all = outer.enter_context(nc.sbuf_tensor("gates_all", [128, NT, 2], F32))

    with Phase(nc, "p1") as ph:
        unT = ph.sb("unT", [128, 8, T], BF16)
        gB = ph.sb("gB", [128, D], F32)
        cstf = ph.sb("cstf", [128, 128], F32)
        identb = ph.sb("identb", [128, 128], BF16)
        xt = [ph.sb(f"xt{i}", [128, D], F32) for i in range(2)]
        xn = [ph.sb(f"xn{i}", [128, D], BF16) for i in range(2)]
        junk = ph.sb("junk", [128, D], BF16)
        ss = ph.sb("ss", [128, NT], F32)
        rt = ph.sb("rt", [128, NT], F32)
        rstd = ph.sb("rstd", [128, NT], F32)
        wst = [ph.sb(f"wst{i}", [128, 8, 512], F32) for i in range(2)]
        wbf = [ph.sb(f"wbf{i}", [128, 8, 512], BF16) for i in range(2)]
        tb = [ph.sb(f"tb{i}", [128, 2, 512], F32) for i in range(3)]
        t1 = [ph.sb(f"t1_{i}", [128, 512], F32) for i in range(2)]
        t2 = [ph.sb(f"t2_{i}", [128, 512], F32) for i in range(2)]
        ost = [ph.sb(f"ost{i}", [128, 512], BF16) for i in range(4)]
        osf = [ph.sb(f"osf{i}", [128, 8], F32) for i in range(2)]
        pT = [ph.ps(f"pT{i}", [128, 1024], BF16) for i in range(2)]
        pp = [ph.ps(f"pp{i}", [128, 512], F32) for i in range(4)]

        ph.dma(gB[:, :], g1[0:1, :].partition_broadcast(128), w=["gB"])
        ph.dma(cstf[:, :], cst[:, C_ID:C_ID + 128], w=["cstf"])
        ph.op("pool", lambda e: e.tensor_copy(out=identb[:, :], in_=cstf[:, :]), r=["cstf"], w=["identb"])

        for i in range(NT):
            x_, xk = xt[i % 2], ("xt", i % 2)
            n_, nk = xn[i % 2], ("xn", i % 2)
            p_, pk = pT[i % 2], ("pT", i % 2)
            ph.dma(x_[:, :], h0[i * 128:(i + 1) * 128, :], w=[xk])
            ph.op("act", lambda e, x_=x_, i=i: e.activation(out=junk[:, :], in_=x_[:, :], func=AF.Square,
                                                          accum_out=ss[:, i:i + 1]),
                  r=[xk], w=["junk", ("ss", i)])
            ph.op("act", lambda e, i=i: e.activation(out=rt[:, i:i + 1], in_=ss[:, i:i + 1], func=AF.Sqrt,
                                                    bias=EPS, scale=1.0 / D),
                  r=[("ss", i)], w=[("rt", i)])
            ph.op("dve", lambda e, i=i: e.reciprocal(out=rstd[:, i:i + 1], in_=rt[:, i:i + 1]),
                  r=[("rt", i)], w=[("rstd", i)])
            ph.op("dve", lambda e, x_=x_, n_=n_, i=i: e.scalar_tensor_tensor(
                out=n_[:, :], in0=x_[:, :], scalar=rstd[:, i:i + 1], in1=gB[:, :], op0=ALU.mult, op1=ALU.mult),
                r=[xk, ("rstd", i), "gB"], w=[nk])

            def tr(e, n_=n_, p_=p_):
                ins = None
                for c in range(8):
                    ins = e.transpose(out=p_[:, c * 128:(c + 1) * 128], in_=n_[:, c * 128:(c + 1) * 128],
                                      identity=identb[:, :])
                return ins
            ph.op("pe", tr, r=[nk, "identb"], w=[pk])
            ph.op("act", lambda e, p_=p_, i=i: e.copy(
                out=unT[:, :, i * 128:(i + 1) * 128], in_=p_[:, :].rearrange("p (c t) -> p c t", c=8)),
                r=[pk], w=[("unT", i)])
        allun = [("unT", i) for i in range(NT)]
        for c in range(8):
            ph.dma(unT_d[:, c, :], unT[:, c, :], r=allun)

        wring = Ring("w", list(zip(wst, wbf)))
        ppr = Ring("pp", pp)
        ostr = Ring("ost", ost)
        tbr = Ring("tb", tb)
        t1r = Ring("t1", t1)
        t2r = Ring("t2", t2)
        osfr = Ring("osf", osf)

        def load_group(src, c0, n):
            (ws, wb), wk = wring.next()
            ph.dma(ws[:, :, :n], src[:, c0:c0 + n].rearrange("(c p) n -> p c n", p=128), w=[("wst",) + wk])
            ph.op("act", lambda e: e.copy(out=wb[:, :, :n], in_=ws[:, :, :n]),
                  r=[("wst",) + wk], w=[("wbf",) + wk])
            return wb, ("wbf",) + wk

        def mm8(e, ps_ap, lhs_fn, rhs_fn):
            ins = None
            for c in range(8):
                ins = e.matmul(ps_ap, lhsT=lhs_fn(c), rhs=rhs_fn(c), start=(c == 0), stop=(c == 7))
            return ins

        def fm_jobs(src, c0, n, jobs):
            wb, wk = load_group(src, c0, n)
            for (t0, w) in blocks512:
                tiles = [("unT", t0 // 128 + j) for j in range(w // 128)]
                for (lc, dest, drow, mode, arg) in jobs:
                    o_, ok = ostr.next()
                    if mode == "plain":
                        p_, pk = ppr.next()
                        ph.op("pe", lambda e, p_=p_, lc=lc, t0=t0, w=w: mm8(
                            e, p_[:, :w], lambda c: wb[:, c, lc:lc + 128], lambda c: unT[:, c, t0:t0 + w]),
                            r=[wk] + tiles, w=[pk])
                        ph.op("act", lambda e, p_=p_, o_=o_, w=w, arg=arg: e.activation(
                            out=o_[:, :w], in_=p_[:, :w], func=AF.Copy, scale=float(arg)), r=[pk], w=[ok])
                    else:
                        tw, ts = arg
                        p_, pk = ppr.next()
                        q_, qk = ppr.next()
                        tb_, tk = tbr.next()
                        a_, ak = t1r.next()
                        b_, bk = t2r.next()
                        ph.dma(tb_[:, :, :w], tabs[2 * ts:2 * ts + 2, :, t0:t0 + w].rearrange("a p t -> p a t"), w=[tk])
                        ph.op("pe", lambda e, p_=p_, lc=lc, t0=t0, w=w: mm8(
                            e, p_[:, :w], lambda c: wb[:, c, lc:lc + 128], lambda c: unT[:, c, t0:t0 + w]),
                            r=[wk] + tiles, w=[pk])
                        ph.op("pe", lambda e, q_=q_, tw=tw, t0=t0, w=w: mm8(
                            e, q_[:, :w], lambda c: wb[:, c, tw:tw + 128], lambda c: unT[:, c, t0:t0 + w]),
                            r=[wk] + tiles, w=[qk])
                        ph.op("dve", lambda e, a_=a_, p_=p_, tb_=tb_, w=w: e.tensor_tensor(
                            out=a_[:, :w], in0=p_[:, :w], in1=tb_[:, 0, :w], op=ALU.mult), r=[pk, tk], w=[ak])
                        ph.op("dve", lambda e, b_=b_, q_=q_, tb_=tb_, w=w: e.tensor_tensor(
                            out=b_[:, :w], in0=q_[:, :w], in1=tb_[:, 1, :w], op=ALU.mult), r=[qk, tk], w=[bk])
                        ph.op("pool", lambda e, a_=a_, b_=b_, o_=o_, w=w: e.tensor_tensor(
                            out=o_[:, :w], in0=a_[:, :w], in1=b_[:, :w], op=ALU.add), r=[ak, bk], w=[ok])
                    ph.dma(dest[drow:drow + 128, t0:t0 + w], o_[:, :w], r=[ok])

        fm_jobs(winA, O_QA, 512, [(m * 128, QA_d, m * 128, "plain", 0.125) for m in range(4)])
        fm_jobs(winA, O_KA, 512, [(m * 128, KA_d, m * 128, "plain", 1.0) for m in range(4)])
        fm_jobs(winB, B_QBN, 512, [(m * 128, QBN_d, m * 128, "plain", 1.0) for m in range(4)])
        fm_jobs(winB, B_QBR, 512, [(m * 128, QBR_d, m * 128, "rope", (256 + m * 128, 0)) for m in range(2)])
        fm_jobs(winB, B_KR, 512, [(0, KR_d, 0, "rope", (128, 0)), (256, KI_d, 0, "rope", (384, 1))])
        fm_jobs(winB, B_QI0, 512, [(m * 128, QI_d, m * 128, "rope", (256 + m * 128, 1)) for m in range(2)])
        fm_jobs(winB, B_QI1, 512, [(m * 128, QI_d, 256 + m * 128, "rope", (256 + m * 128, 1)) for m in range(2)])
        fm_jobs(winB, B_CKV, 128, [(0, CKVT_d, 0, "plain", 1.0)])

        wb, wk = load_group(winA, O_VA, 512)
        for i in range(NT):
            p_, pk = ppr.next()
            o_, ok = ostr.next()
            ph.op("pe", lambda e, p_=p_, i=i, wb=wb: mm8(
                e, p_[:, :], lambda c: unT[:, c, i * 128:(i + 1) * 128], lambda c: wb[:, c, 0:512]),
                r=[wk, ("unT", i)], w=[pk])
            ph.op("act", lambda e, p_=p_, o_=o_: e.copy(out=o_[:, :], in_=p_[:, :]), r=[pk], w=[ok])
            ph.dma(VA_d[i * 128:(i + 1) * 128, :], o_[:, :], r=[ok])
        wb, wk = load_group(winB, B_CKV, 136)
        for i in range(NT):
            p_, pk = ppr.next()
            o_, ok = ostr.next()
            f_, fk = osfr.next()
            ph.op("pe", lambda e, p_=p_, i=i, wb=wb: mm8(
                e, p_[:, :136], lambda c: unT[:, c, i * 128:(i + 1) * 128], lambda c: wb[:, c, 0:136]),
                r=[wk, ("unT", i)], w=[pk])
            ph.op("act", lambda e, p_=p_, o_=o_: e.copy(out=o_[:, :128], in_=p_[:, :128]), r=[pk], w=[ok])
            ph.op("act", lambda e, p_=p_, f_=f_: e.copy(out=f_[:, :], in_=p_[:, 128:136]), r=[pk], w=[fk])
            ph.dma(CKV_d[i * 128:(i + 1) * 128, :], o_[:, :128], r=[ok])
            ph.dma(WI_d[i * 128:(i + 1) * 128, :], f_[:, :], r=[fk])


    if stop_after < 2:
        return nc

    with Phase(nc, "p2") as ph:
        cs = ph.sb("cs", [128, C_SBM + 3 * QB], F32)
        TNb = ph.sb("TNb", [128, 128], BF16)
        ONb = ph.sb("ONb", [128, 128], BF16)
        SBMb = ph.sb("SBMb", [128, 3, QB], BF16)
        Vt = ph.sb("Vt", [128, NT, 512], BF16)
        QT = [ph.sb(f"QT{i}", [128, T], BF16) for i in range(2)]
        KT = [ph.sb(f"KT{i}", [128, T], BF16) for i in range(2)]
        Es = [ph.sb(f"Es{i}", [128, QB], F32) for i in range(6)]
        SPs = [ph.sb(f"SP{i}", [128, QB], BF16) for i in range(6)]
        Xs = [ph.sb(f"X{i}", [128, QB], F32) for i in range(3)]
        As = [ph.sb(f"A{i}", [128, QB], BF16) for i in range(4)]
        ACC = [ph.sb(f"ACC{i}", [128, QB], BF16) for i in range(3)]
        yst = [ph.sb(f"yst{i}", [128, QB], BF16) for i in range(2)]
        pS = [ph.ps(f"pS{i}", [128, 512]) for i in range(3)]
        pL = [ph.ps(f"pL{i}", [128, 512]) for i in range(3)]
        pY = [ph.ps(f"pY{i}", [128, 512]) for i in range(2)]
        ph.dma(cs[:, :], cst[:, 0:C_SBM + 3 * QB], w=["cs"])
        ph.op("pool", lambda e: e.tensor_copy(out=TNb[:, :], in_=cs[:, C_TN:C_TN + 128]), r=["cs"], w=["TNb"])
        ph.op("pool", lambda e: e.tensor_copy(out=ONb[:, :], in_=cs[:, C_ON:C_ON + 128]), r=["cs"], w=["ONb"])
        ph.op("pool", lambda e: e.tensor_copy(out=SBMb[:, :, :].rearrange("p m q -> p (m q)"),
                                              in_=cs[:, C_SBM:C_SBM + 3 * QB]), r=["cs"], w=["SBMb"])
        for g in range(3):
            ph.dma(Vt[:, g * 11:(g + 1) * 11, :],
                   VA_d[g * 11 * 128:(g + 1) * 11 * 128, :].rearrange("(i p) n -> p i n", p=128), w=[("Vt", g)])
        vkeys = [("Vt", g) for g in range(3)]
        Er, SPr, Xr, Ar = Ring("E", Es), Ring("SP", SPs), Ring("X", Xs), Ring("A", As)
        pSr, pLr, pYr = Ring("pS", pS), Ring("pL", pL), Ring("pY", pY)
        ACCr, ystr = Ring("ACC", ACC), Ring("yst", yst)

        def load_pair(p):
            ph.dma(QT[p % 2][:, :], QA_d[p * 128:(p + 1) * 128, :], w=[("QT", p % 2)])
            ph.dma(KT[p % 2][:, :], KA_d[p * 128:(p + 1) * 128, :], w=[("KT", p % 2)])

        units = []
        for h in range(8):
            for b in range(NQB):
                grp = {"h": h, "b": b}
                jl = list(range(3 * b + 2, -1, -1))
                for j in jl:
                    units.append({"g": grp, "j": j, "first": j == jl[0], "last": j == 0})

        def stageA(u):
            g = u["g"]
            h, b, j = g["h"], g["b"], u["j"]
            p = h // 2
            if u["first"] and b == 0 and h % 2 == 0:
                if p == 0:
                    load_pair(0)
                if p + 1 < 4:
                    load_pair(p + 1)
            hr = slice((h % 2) * 64, (h % 2) * 64 + 64)
            Q_, K_ = QT[p % 2], KT[p % 2]
            qk, kk = ("QT", p % 2), ("KT", p % 2)
            q0 = b * QB
            m = j - 3 * b
            c0 = 128 * m if m > 0 else 0
            cw = slice(c0, QB)
            s_, sk = pSr.next()
            E_, ek = Er.next()
            SP_, spk = SPr.next()
            u.update(hr=hr, cw=cw, E=E_, ek=ek, SP=SP_, spk=spk)
            ph.op("pe", lambda e: e.matmul(s_[:, cw], lhsT=K_[hr, j * 128:(j + 1) * 128], rhs=Q_[hr, q0 + c0:q0 + QB],
                                           start=True, stop=True), r=[qk, kk], w=[sk])
            ph.op("act", lambda e: e.activation(out=E_[:, cw], in_=s_[:, cw], func=AF.Exp), r=[sk], w=[ek])
            ph.op("act", lambda e: e.activation(out=SP_[:, cw], in_=E_[:, cw], func=AF.Ln, bias=1.0), r=[ek], w=[spk])
            if m >= 0:
                ph.op("pool", lambda e: e.tensor_tensor(out=SP_[:, cw], in0=SP_[:, cw], in1=SBMb[:, m, cw], op=ALU.mult),
                      r=[spk, "SBMb"], w=[spk])
                ph.op("pool", lambda e: e.tensor_tensor(out=E_[:, cw], in0=E_[:, cw], in1=SBMb[:, m, cw], op=ALU.mult),
                      r=[ek, "SBMb"], w=[ek])

        def stageB(u):
            g = u["g"]
            cw, E_, ek, SP_, spk = u["cw"], u["E"], u["ek"], u["SP"], u["spk"]
            if u["first"]:
                acc, acck = ACCr.next()
                g["acc"], g["acck"] = acc, acck
                ph.op("pool", lambda e: e.memset(acc[:, :], 0.0), w=[acck])
            acc, acck = g["acc"], g["acck"]
            l_, lk = pLr.next()
            X_, xk2 = Xr.next()
            A_, ak = Ar.next()
            u.update(A=A_, ak=ak)

            def lmm(e):
                e.matmul(l_[:, cw], lhsT=TNb[:, :], rhs=SP_[:, cw], start=True, stop=False)
                return e.matmul(l_[:, cw], lhsT=ONb[:, :], rhs=acc[:, cw], start=False, stop=True)
            ph.op("pe", lmm, r=[spk, acck, "TNb", "ONb"], w=[lk])
            ph.op("act", lambda e: e.activation(out=X_[:, cw], in_=l_[:, cw], func=AF.Exp), r=[lk], w=[xk2])
            ph.op("dve", lambda e: e.tensor_tensor(out=A_[:, cw], in0=X_[:, cw], in1=E_[:, cw], op=ALU.mult),
                  r=[xk2, ek], w=[ak])
            ph.op("pool", lambda e: e.tensor_tensor(out=acc[:, cw], in0=acc[:, cw], in1=SP_[:, cw], op=ALU.add),
                  r=[acck, spk], w=[acck])

        def stageC(u):
            g = u["g"]
            h, b, j = g["h"], g["b"], u["j"]
            hr, cw, A_, ak = u["hr"], u["cw"], u["A"], u["ak"]
            if u["first"]:
                g["y"], g["yk"] = pYr.next()
            y_, yk = g["y"], g["yk"]
            first, last = u["first"], u["last"]
            ph.op("pe", lambda e: e.matmul(y_[hr, cw], lhsT=Vt[:, j, h * 64:(h + 1) * 64], rhs=A_[:, cw],
                                           start=first, stop=last), r=[ak] + vkeys, w=[yk])
            if last:
                o_, ok = ystr.next()
                q0 = b * QB
                ph.op("act", lambda e: e.copy(out=o_[hr, :], in_=y_[hr, :QB]), r=[yk], w=[ok])
                ph.dma(YA_d[h * 64:(h + 1) * 64, q0:q0 + QB], o_[hr, :], r=[ok])

        SKB, SKC = 2, 3
        n = len(units)
        for t in range(n + SKC):
            if t < n:
                stageA(units[t])
            if 0 <= t - SKB < n:
                stageB(units[t - SKB])
            if 0 <= t - SKC < n:
                stageC(units[t - SKC])

    if stop_after < 3:
        return nc

    with Phase(nc, "p3") as ph:
        cs = ph.sb("cs", [128, NCST], F32)
        identb = ph.sb("identb", [128, 128], BF16)
        OPb = ph.sb("OPb", [128, 128], BF16)
        wukf = ph.sb("wukf", [128, 4, 128], F32)
        wukb = ph.sb("wukb", [128, 4, 128], BF16)
        wuvf = ph.sb("wuvf", [128, 8, 64], F32)
        wuvb = ph.sb("wuvb", [128, 8, 64], BF16)
        CKVT = ph.sb("CKVT", [128, T], BF16)
        CKVt = ph.sb("CKVt", [128, NT, 128], BF16)
        KR = ph.sb("KR", [128, T], BF16)
        KI = ph.sb("KI", [128, T], BF16)
        QIb = [ph.sb(f"QIb{i}", [128, 4, QB], BF16) for i in range(2)]
        QBNb = [ph.sb(f"QBNb{i}", [128, 4, QB], BF16) for i in range(2)]
        QBRb = [ph.sb(f"QBRb{i}", [128, 2, QB], BF16) for i in range(2)]
        WIb = [ph.sb(f"WIb{i}", [128, 3, 8], F32) for i in range(2)]
        dg = [ph.sb(f"dg{i}", [128, 8, 128], BF16) for i in range(2)]
        Rr_ = [ph.sb(f"R{i}", [128, 512], BF16) for i in range(3)]
        score = [ph.sb(f"score{i}", [128, T], F32) for i in range(2)]
        junkb = ph.sb("junkb", [128, T], BF16)
        mb = [ph.sb(f"mb{i}", [128, 3, T], BF16) for i in range(2)]
        sm = [ph.sb(f"sm{i}", [128, 8 + 2 * (NITER + 2)], F32) for i in range(2)]
        steps = [ph.sb(f"steps{i}", [128, NITER + 1], F32) for i in range(2)]
        qlat = [ph.sb(f"qlat{i}", [128, QB], BF16) for i in range(2)]
        Pb = [ph.sb(f"P{i}", [128, QB], BF16) for i in range(3)]
        numS = [ph.sb(f"numS{i}", [128, QB], F32) for i in range(2)]
        denS = [ph.sb(f"denS{i}", [128, QB], F32) for i in range(2)]
        olat = [ph.sb(f"olat{i}", [128, QB], BF16) for i in range(2)]
        yst = [ph.sb(f"yst{i}", [128, QB], BF16) for i in range(2)]
        pI = [ph.ps(f"pI{i}", [128, 512]) for i in range(2)]
        pSc = [ph.ps("pSc0", [128, 512])]
        pLg = [ph.ps(f"pLg{i}", [128, 512]) for i in range(2)]
        pN = ph.ps("pN", [128, 512])
        pD = ph.ps("pD", [128, 512])
        pQY = ph.ps("pQY", [128, 512])

        ph.dma(cs[:, :], cst[:, :], w=["cs"])
        ph.op("pool", lambda e: e.tensor_copy(out=identb[:, :], in_=cs[:, C_ID:C_ID + 128]), r=["cs"], w=["identb"])
        ph.op("pool", lambda e: e.tensor_copy(out=OPb[:, :], in_=cs[:, C_OP:C_OP + 128]), r=["cs"], w=["OPb"])
        ph.dma(wukf[:, :, :], wukT[:, :, :], w=["wukf"])
        ph.op("pool", lambda e: e.tensor_copy(out=wukb[:, :, :], in_=wukf[:, :, :]), r=["wukf"], w=["wukb"])
        ph.dma(wuvf[:, :, :], wuv[:, :, :], w=["wuvf"])
        ph.op("pool", lambda e: e.tensor_copy(out=wuvb[:, :, :], in_=wuvf[:, :, :]), r=["wuvf"], w=["wuvb"])
        ph.dma(CKVT[:, :], CKVT_d[:, :], w=["CKVT"])
        ph.dma(KR[:, :], KR_d[:, :], w=["KR"])
        ph.dma(KI[:, :], KI_d[:, :], w=["KI"])
        for g in range(3):
            ph.dma(CKVt[:, g * 11:(g + 1) * 11, :],
                   CKV_d[g * 11 * 128:(g + 1) * 11 * 128, :].rearrange("(i p) n -> p i n", p=128), w=[("CKVt", g)])
        ckeys = [("CKVt", g) for g in range(3)]
        pIr, pLgr, Rr, Pr = Ring("pI", pI), Ring("pLg", pLg), Ring("R", Rr_), Ring("P", Pb)
        qlr, numr, denr, olr, ystr = Ring("qlat", qlat), Ring("numS", numS), Ring("denS", denS), Ring("olat", olat), Ring("yst", yst)
        scr, dgr, smr, stpr = Ring("score", score), Ring("dg", dg), Ring("sm", sm), Ring("steps", steps)

        def load_block(b):
            q0 = b * QB
            i2 = b % 2
            ph.dma(QIb[i2][:, :, :], QI_d[:, q0:q0 + QB].rearrange("(c p) q -> p c q", p=128), w=[("QIb", i2)])
            ph.dma(QBNb[i2][:, :, :], QBN_d[:, q0:q0 + QB].rearrange("(c p) q -> p c q", p=128), w=[("QBNb", i2)])
            ph.dma(QBRb[i2][:, :, :], QBR_d[:, q0:q0 + QB].rearrange("(c p) q -> p c q", p=128), w=[("QBRb", i2)])
            ph.dma(WIb[i2][:, :, :], WI_d[q0:q0 + QB, :].rearrange("(s p) h -> p s h", p=128), w=[("WIb", i2)])

        def gen_index(b):
            i2 = b % 2
            mb_, mbk = mb[i2], ("mb", i2)
            ph.op("pool", lambda e: e.memset(mb_[:, :, :], -30000.0), w=[mbk])
            yield

            def tile_gen(s):
                i = 3 * b + s
                nkt = min(i + 2, NT)
                Nk = nkt * 128
                sc, sck = scr.next()
                dg_, dgk = dgr.next()
                sm_, smk = smr.next()
                st_, stk = stpr.next()
                for h in range(8):
                    ph.op("act", lambda e, dg_=dg_, h=h, s=s: e.activation(
                        out=dg_[:, h, :], in_=identb[:, :], func=AF.Copy, scale=WIb[i2][:, s, h:h + 1]),
                        r=["identb", ("WIb", i2)], w=[dgk])
                a_, ak = pSc[0], ("pSc", 0)
                items = [(k0, min(512, Nk - k0), h) for k0 in range(0, Nk, 512) for h in range(8)]
                slot = {}

                def rec_I(n):
                    k0, w, h = items[n]
                    hr = slice((h % 2) * 64, (h % 2) * 64 + 64)
                    p_, pk = pIr.next()
                    slot[n] = (p_, pk)
                    ph.op("pe", lambda e: e.matmul(
                        p_[:, :w], lhsT=QIb[i2][hr, h // 2, s * 128:(s + 1) * 128], rhs=KI[hr, k0:k0 + w],
                        start=True, stop=True), r=[("QIb", i2), "KI"], w=[pk])

                def rec_RD(n):
                    k0, w, h = items[n]
                    p_, pk = slot.pop(n)
                    r_, rk = Rr.next()
                    ph.op("act", lambda e: e.activation(out=r_[:, :w], in_=p_[:, :w], func=AF.Relu), r=[pk], w=[rk])
                    ph.op("pe", lambda e: e.matmul(a_[:, :w], lhsT=dg_[:, h, :], rhs=r_[:, :w], start=(h == 0), stop=(h == 7)),
                          r=[dgk, rk], w=[ak])
                    if h == 7:
                        ph.op("act", lambda e: e.copy(out=sc[:, k0:k0 + w], in_=a_[:, :w]), r=[ak], w=[sck])

                rec_I(0)
                for n in range(len(items)):
                    if n + 1 < len(items):
                        rec_I(n + 1)
                    rec_RD(n)
                    if items[n][2] == 7:
                        yield
                ph.op("dve", lambda e, sc=sc, sm_=sm_, Nk=Nk: e.tensor_reduce(
                    out=sm_[:, 0:1], in_=sc[:, :Nk], axis=AX.X, op=ALU.max), r=[sck], w=[smk])
                ph.op("dve", lambda e, sc=sc, sm_=sm_, Nk=Nk: e.tensor_reduce(
                    out=sm_[:, 1:2], in_=sc[:, :Nk], axis=AX.X, op=ALU.min), r=[sck], w=[smk])
                ph.op("pool", lambda e, sc=sc, i=i: e.tensor_tensor(
                    out=sc[:, i * 128:(i + 1) * 128], in0=sc[:, i * 128:(i + 1) * 128], in1=cs[:, C_MD:C_MD + 128],
                    op=ALU.add), r=[sck, "cs"], w=[sck])
                if i + 1 < NT:
                    ph.op("pool", lambda e, sc=sc, i=i: e.tensor_tensor(
                        out=sc[:, (i + 1) * 128:(i + 2) * 128], in0=sc[:, (i + 1) * 128:(i + 2) * 128],
                        in1=cs[:, C_MN:C_MN + 128], op=ALU.add), r=[sck, "cs"], w=[sck])
                ph.op("dve", lambda e, sm_=sm_: e.tensor_tensor(out=sm_[:, 6:7], in0=sm_[:, 0:1], in1=sm_[:, 1:2],
                                                               op=ALU.subtract), r=[smk], w=[smk])
                ph.op("dve", lambda e, sm_=sm_: e.tensor_scalar(out=sm_[:, 7:8], in0=sm_[:, 6:7], scalar1=-1.0 / 64, scalar2=-1e-6,
                                                               op0=ALU.mult, op1=ALU.add), r=[smk], w=[smk])
                ph.op("dve", lambda e, sm_=sm_: e.tensor_tensor(out=sm_[:, 1:2], in0=sm_[:, 1:2], in1=sm_[:, 7:8],
                                                               op=ALU.add), r=[smk], w=[smk])
                ph.op("dve", lambda e, sm_=sm_: e.tensor_tensor(out=sm_[:, 2:3], in0=sm_[:, 0:1], in1=sm_[:, 1:2],
                                                               op=ALU.subtract), r=[smk], w=[smk])
                ph.op("dve", lambda e, sm_=sm_, st_=st_: e.tensor_scalar(
                    out=st_[:, :], in0=cs[:, C_P2:C_P2 + NITER + 1], scalar1=sm_[:, 2:3], scalar2=None, op0=ALU.mult),
                    r=[smk, "cs"], w=[stk])
                ph.op("dve", lambda e, sm_=sm_, st_=st_: e.tensor_tensor(out=sm_[:, 8:9], in0=sm_[:, 1:2], in1=st_[:, 0:1],
                                                                        op=ALU.add), r=[smk, stk], w=[smk])
                yield
                for it in range(NITER):
                    mid = sm_[:, 8 + it:9 + it]
                    mid2 = sm_[:, 9 + it:10 + it]
                    cnt = sm_[:, 3:4]
                    g_ = sm_[:, 4:5]
                    ph.op("dve", lambda e, sc=sc, mid=mid, cnt=cnt, Nk=Nk: e.tensor_scalar(
                        out=junkb[:, :Nk], in0=sc[:, :Nk], scalar1=mid, scalar2=None, op0=ALU.is_ge, op1=ALU.add,
                        accum_out=cnt), r=[sck, smk], w=[smk, "junkb"])
                    ph.op("dve", lambda e, cnt=cnt, g_=g_, st_=st_, it=it: e.tensor_scalar(
                        out=g_, in0=cnt, scalar1=TOPK - 0.5, scalar2=st_[:, it:it + 1], op0=ALU.is_ge, op1=ALU.mult),
                        r=[smk, stk], w=[smk])
                    ph.op("dve", lambda e, g_=g_, mid=mid, mid2=mid2, st_=st_, it=it: e.scalar_tensor_tensor(
                        out=mid2, in0=g_, scalar=st_[:, it + 1:it + 2], in1=mid, op0=ALU.subtract, op1=ALU.add),
                        r=[smk, stk], w=[smk])
                    yield
                lo = sm_[:, 5:6]
                ph.op("dve", lambda e, sm_=sm_, st_=st_, lo=lo: e.tensor_tensor(
                    out=lo, in0=sm_[:, 8 + NITER:9 + NITER], in1=st_[:, NITER:NITER + 1], op=ALU.subtract),
                    r=[smk, stk], w=[smk])
                ph.op("dve", lambda e, sc=sc, lo=lo, s=s, Nk=Nk: e.tensor_scalar(
                    out=mb_[:, s, :Nk], in0=sc[:, :Nk], scalar1=lo, scalar2=-30000.0, op0=ALU.is_lt, op1=ALU.mult),
                    r=[sck, smk], w=[mbk])
                yield

            for s in range(3):
                yield from tile_gen(s)

        def gen_attn(b):
            q0 = b * QB
            i2 = b % 2
            mb_, mbk = mb[i2], ("mb", i2)
            nkb = min(3 * b + 4, NT)
            pend = []

            def unit(h, j, ql, qlk):
                hr = slice((h % 2) * 64, (h % 2) * 64 + 64)
                g4 = h % 4
                r32 = slice(32 * g4, 32 * g4 + 32)
                lg, lgk = pLgr.next()
                P_, pk = Pr.next()
                ks = slice(j * 128, (j + 1) * 128)

                def lmm(e):
                    e.matmul(lg[:, :QB], lhsT=CKVT[:, ks], rhs=ql[:, :], start=True, stop=False)
                    e.matmul(lg[:, :QB], lhsT=KR[r32, ks], rhs=QBRb[i2][r32, h // 4, :], start=False, stop=False,
                             tile_position=(32 * g4, 0))
                    ins = None
                    for s in range(3):
                        ins = e.matmul(lg[:, s * 128:(s + 1) * 128], lhsT=mb_[:, s, ks], rhs=identb[:, :],
                                       start=False, stop=(s == 2))
                    return ins
                ph.op("pe", lmm, r=["CKVT", "KR", qlk, ("QBRb", i2), mbk, "identb"], w=[lgk])
                ph.op("act", lambda e: e.activation(out=P_[:, :], in_=lg[:, :QB], func=AF.Exp, scale=0.125), r=[lgk], w=[pk])

                def stage_b():
                    first, last = (j == 0), (j == nkb - 1)

                    def avmm(e):
                        e.matmul(pN[:, :QB], lhsT=CKVt[:, j, :], rhs=P_[:, :], start=first, stop=last)
                        return e.matmul(pD[:, :QB], lhsT=OPb[:, :], rhs=P_[:, :], start=first, stop=last)
                    ph.op("pe", avmm, r=[pk, "OPb"] + ckeys, w=["pN", "pD"])
                    if last:
                        nS, nk_ = numr.next()
                        dS, dk_ = denr.next()
                        ol, olk = olr.next()
                        ph.op("act", lambda e: e.copy(out=nS[:, :], in_=pN[:, :QB]), r=["pN"], w=[nk_])
                        ph.op("act", lambda e: e.copy(out=dS[:, :], in_=pD[:, :QB]), r=["pD"], w=[dk_])
                        ph.op("dve", lambda e: e.reciprocal(out=dS[:, :], in_=dS[:, :]), r=[dk_], w=[dk_])
                        ph.op("dve", lambda e: e.tensor_tensor(out=ol[:, :], in0=nS[:, :], in1=dS[:, :], op=ALU.mult),
                              r=[nk_, dk_], w=[olk])
                        ph.op("pe", lambda e: e.matmul(pQY[hr, :QB], lhsT=wuvb[:, h, :], rhs=ol[:, :], start=True, stop=True),
                              r=[olk, "wuvb"], w=["pQY"])
                        o_, ok = ystr.next()
                        ph.op("act", lambda e: e.copy(out=o_[hr, :], in_=pQY[hr, :QB]), r=["pQY"], w=[ok])
                        ph.dma(YB_d[h * 64:(h + 1) * 64, q0:q0 + QB], o_[hr, :], r=[ok])
                return stage_b

            def qlat_for(h):
                hr = slice((h % 2) * 64, (h % 2) * 64 + 64)
                ql, qlk = qlr.next()
                ph.op("pe", lambda e: e.matmul(pQY[:, :QB], lhsT=wukb[hr, h // 2, :], rhs=QBNb[i2][hr, h // 2, :],
                                               start=True, stop=True), r=["wukb", ("QBNb", i2)], w=["pQY"])
                ph.op("act", lambda e: e.copy(out=ql[:, :], in_=pQY[:, :QB]), r=["pQY"], w=[qlk])
                return ql, qlk

            for h in range(8):
                ql, qlk = qlat_for(h)
                for j in range(nkb):
                    sb_ = unit(h, j, ql, qlk)
                    if pend:
                        pend.pop(0)()
                    pend.append(sb_)
                    yield
            while pend:
                pend.pop(0)()
            yield

        def run_gens(gens):
            items = []
            for g, n in gens:
                items.append([g, n, 0])
            while items:
                items.sort(key=lambda t: t[2] / t[1])
                it = items[0]
                try:
                    next(it[0])
                    it[2] += 1
                except StopIteration:
                    items.remove(it)

        def n_index(b):
            n = 1
            for s in range(3):
                i = 3 * b + s
                Nk = min(i + 2, NT) * 128
                n += (Nk + 511) // 512 + 1 + NITER + 1
            return n

        load_block(0)
        run_gens([(gen_index(0), n_index(0))])
        for b in range(NQB):
            gens = [(gen_attn(b), 8 * (min(3 * b + 4, NT) + 1))]
            if b + 1 < NQB:
                load_block(b + 1)
                gens.append((gen_index(b + 1), n_index(b + 1)))
            run_gens(gens)


    if stop_after < 4:
        outer.close()
        return nc

    with Phase(nc, "p4") as ph:
        cs = ph.sb("cs", [128, NCST], F32)
        iocap = ph.sb("iocap", [128, 32], F32)
        g2B = ph.sb("g2B", [128, D], F32)
        brB = ph.sb("brB", [128, 36], F32)
        wrf = ph.sb("wrf", [128, 8, 36], F32)
        wgab = ph.sb("wgab", [128, 8, D], BF16)
        wgbb = ph.sb("wgbb", [128, 8, D], BF16)
        wuab = ph.sb("wuab", [128, 4, D], BF16)
        wubb = ph.sb("wubb", [128, 4, D], BF16)
        wob = ph.sb("wob", [128, 8, D], BF16)
        wst = [ph.sb(f"wst{i}", [128, 8, 512], F32) for i in range(2)]
        unb = [ph.sb(f"unb{i}", [128, 8, QB], BF16) for i in range(2)]
        yab = [ph.sb(f"yab{i}", [128, 4, QB], BF16) for i in range(2)]
        ybb = [ph.sb(f"ybb{i}", [128, 4, QB], BF16) for i in range(2)]
        sg = [ph.sb(f"sg{i}", [128, QB], F32) for i in range(4)]
        zab = [ph.sb(f"zab{i}", [128, QB], F32) for i in range(4)]
        zT = [ph.sb(f"zT{i}", [128, 8, QB], BF16) for i in range(2)]
        xt = [ph.sb(f"xt{i}", [128, D], F32) for i in range(2)]
        h1t = [ph.sb(f"h1t{i}", [128, D], F32) for i in range(2)]
        u2f = [ph.sb(f"u2f{i}", [128, D], F32) for i in range(2)]
        u2b = [ph.sb(f"u2b{i}", [128, D], BF16) for i in range(2)]
        u2T = [ph.sb(f"u2T{i}", [128, 8, 128], F32) for i in range(2)]
        junk = ph.sb("junk", [128, D], BF16)
        rs = [ph.sb(f"rs{i}", [128, 128], F32) for i in range(2)]
        ACCA = ph.sb("ACCA", [128, 32], F32)
        sli = [ph.sb(f"sli{i}", [128, 2], I32) for i in range(2)]
        lgs = [ph.sb(f"lgs{i}", [128, 36], F32) for i in range(2)]
        asum = [ph.sb(f"asum{i}", [128, 32], F32) for i in range(2)]
        cum = [ph.sb(f"cum{i}", [128, 96], F32) for i in range(2)]
        pr = [ph.ps(f"pr{i}", [128, 512]) for i in range(4)]
        pM = [ph.ps(f"pM{i}", [128, 512]) for i in range(2)]
        pTf = ph.ps("pTf", [128, 1024])

        ph.dma(cs[:, :], cst[:, :], w=["cs"])
        ph.dma(g2B[:, :], g2[0:1, :].partition_broadcast(128), w=["g2B"])
        ph.dma(brB[:, :], br[0:1, :].partition_broadcast(128), w=["brB"])
        ph.dma(wrf[:, :, :], wr[:, :].rearrange("(c p) n -> p c n", p=128), w=["wrf"])
        ph.op("pool", lambda e: e.tensor_scalar(out=iocap[:, :], in0=cs[:, C_IO:C_IO + 32], scalar1=float(CAP), scalar2=None,
                                                op0=ALU.mult), r=["cs"], w=["iocap"])
        ph.op("pool", lambda e: e.memset(ACCA[:, :], 0.0), w=["ACCA"])
        wsr = Ring("wst", wst)

        def load_w(src, nch, dest, dk):
            ncol = src.shape[1]
            for c0 in range(0, ncol, 512):
                ws, wk = wsr.next()
                ph.dma(ws[:, :nch, :], src[:, c0:c0 + 512].rearrange("(c p) n -> p c n", p=128), w=[wk])
                ph.op("act", lambda e, ws=ws, c0=c0: e.copy(out=dest[:, :, c0:c0 + 512], in_=ws[:, :nch, :]),
                      r=[wk], w=[dk])
        load_w(winB[:, B_GA:B_GA + D], 8, wgab, "wgab")
        load_w(winB[:, B_GB:B_GB + D], 8, wgbb, "wgbb")
        load_w(wupa, 4, wuab, "wuab")
        load_w(wupb, 4, wubb, "wubb")
        load_w(wo, 8, wob, "wob")
        prr, sgr, zabr = Ring("pr", pr), Ring("sg", sg), Ring("zab", zab)

        def mmn(e, ps_ap, n, lhs_fn, rhs_fn):
            ins = None
            for c in range(n):
                ins = e.matmul(ps_ap, lhsT=lhs_fn(c), rhs=rhs_fn(c), start=(c == 0), stop=(c == n - 1))
            return ins

        def do_block(b):
            q0 = b * QB
            i2 = b % 2
            ph.dma(unb[i2][:, :, :], unT_d[:, :, q0:q0 + QB], w=[("unb", i2)])
            ph.dma(yab[i2][:, :, :], YA_d[:, q0:q0 + QB].rearrange("(c p) q -> p c q", p=128), w=[("yab", i2)])
            ph.dma(ybb[i2][:, :, :], YB_d[:, q0:q0 + QB].rearrange("(c p) q -> p c q", p=128), w=[("ybb", i2)])
            zk = ("zT", i2)
            for m in range(8):
                ms = slice(m * 128, (m + 1) * 128)
                parts = []
                for (wg, wgk, wu, wuk, yb_, ybk) in ((wgab, "wgab", wuab, "wuab", yab[i2], ("yab", i2)),
                                                     (wgbb, "wgbb", wubb, "wubb", ybb[i2], ("ybb", i2))):
                    pg, pgk = prr.next()
                    pu, puk = prr.next()
                    s_, sk = sgr.next()
                    z_, zk2 = zabr.next()
                    ph.op("pe", lambda e, pg=pg, wg=wg, ms=ms: mmn(e, pg[:, :QB], 8, lambda c: wg[:, c, ms],
                                                                  lambda c: unb[i2][:, c, :]),
                          r=[wgk, ("unb", i2)], w=[pgk])
                    ph.op("act", lambda e, pg=pg, s_=s_: e.activation(out=s_[:, :], in_=pg[:, :QB], func=AF.Sigmoid),
                          r=[pgk], w=[sk])
                    ph.op("pe", lambda e, pu=pu, wu=wu, ms=ms, yb_=yb_: mmn(e, pu[:, :QB], 4, lambda c: wu[:, c, ms],
                                                                          lambda c: yb_[:, c, :]),
                          r=[wuk, ybk], w=[puk])
                    ph.op("dve", lambda e, pu=pu, s_=s_, z_=z_: e.tensor_tensor(out=z_[:, :], in0=pu[:, :QB], in1=s_[:, :],
                                                                               op=ALU.mult), r=[puk, sk], w=[zk2])
                    parts.append((z_, zk2))
                ph.op("pool", lambda e, m=m, parts=parts: e.tensor_tensor(
                    out=zT[i2][:, m, :], in0=parts[0][0][:, :], in1=parts[1][0][:, :], op=ALU.add),
                    r=[parts[0][1], parts[1][1]], w=[zk])
            def do_tile(s):
                i = 3 * b + s
                j2 = i % 2
                ts = slice(s * 128, (s + 1) * 128)
                ph.dma(xt[j2][:, :], h0[i * 128:(i + 1) * 128, :], w=[("xt", j2)])
                for half in range(2):
                    ph.op("pe", lambda e, half=half, ts=ts: mmn(
                        e, pM[half][:, :], 8, lambda c: zT[i2][:, c, ts], lambda c: wob[:, c, half * 512:(half + 1) * 512]),
                        r=[zk, "wob"], w=[("pM", half)])
                    ph.op("dve", lambda e, half=half, j2=j2: e.tensor_tensor(
                        out=h1t[j2][:, half * 512:(half + 1) * 512], in0=pM[half][:, :],
                        in1=xt[j2][:, half * 512:(half + 1) * 512], op=ALU.add),
                        r=[("pM", half), ("xt", j2)], w=[("h1t", j2, half)])
                hk = [("h1t", j2, 0), ("h1t", j2, 1)]
                ph.dma(H1_d[i * 128:(i + 1) * 128, :], h1t[j2][:, :], r=hk)
                R = rs[j2]
                rk = ("rs", j2)
                ph.op("act", lambda e, j2=j2, R=R: e.activation(out=junk[:, :], in_=h1t[j2][:, :], func=AF.Square,
                                                               accum_out=R[:, 0:1]), r=hk, w=["junk", rk])
                ph.op("act", lambda e, R=R: e.activation(out=R[:, 1:2], in_=R[:, 0:1], func=AF.Sqrt, bias=EPS, scale=1.0 / D),
                      r=[rk], w=[rk])
                ph.op("dve", lambda e, R=R: e.reciprocal(out=R[:, 2:3], in_=R[:, 1:2]), r=[rk], w=[rk])
                ph.op("dve", lambda e, j2=j2, R=R: e.scalar_tensor_tensor(
                    out=u2f[j2][:, :], in0=h1t[j2][:, :], scalar=R[:, 2:3], in1=g2B[:, :], op0=ALU.mult, op1=ALU.mult),
                    r=hk + [rk, "g2B"], w=[("u2f", j2)])
                ph.op("pool", lambda e, j2=j2: e.tensor_copy(out=u2b[j2][:, :], in_=u2f[j2][:, :]),
                      r=[("u2f", j2)], w=[("u2b", j2)])

                def trf(e, j2=j2):
                    ins = None
                    for c in range(8):
                        ins = e.transpose(out=pTf[:, c * 128:(c + 1) * 128], in_=u2f[j2][:, c * 128:(c + 1) * 128],
                                          identity=cs[:, C_ID:C_ID + 128])
                    return ins
                ph.op("pe", trf, r=[("u2f", j2), "cs"], w=["pTf"])
                ph.op("act", lambda e, j2=j2: e.copy(out=u2T[j2][:, :, :], in_=pTf[:, :].rearrange("p (c t) -> p c t", c=8)),
                      r=["pTf"], w=[("u2T", j2)])
                pq, pqk = prr.next()
                ph.op("pe", lambda e, pq=pq, j2=j2: mmn(e, pq[:, :36], 8, lambda c: u2T[j2][:, c, :], lambda c: wrf[:, c, :]),
                      r=[("u2T", j2), "wrf"], w=[pqk])
                V = lambda a, b2: R[:, a:b2]
                lgk = ("lg", j2)
                L = lgs[j2]
                ph.op("dve", lambda e, pq=pq, L=L: e.tensor_tensor(out=L[:, :], in0=pq[:, :36], in1=brB[:, :], op=ALU.add),
                      r=[pqk, "brB"], w=[lgk])
                d = lambda fn, r_=(), w_=(): ph.op("dve", fn, r=[rk, lgk] + list(r_), w=[rk] + list(w_))
                d(lambda e, L=L, R=R: e.tensor_reduce(out=R[:, 3:4], in_=L[:, 0:4], axis=AX.X, op=ALU.max))
                d(lambda e, L=L, R=R: e.tensor_scalar(out=R[:, 16:20], in0=L[:, 0:4], scalar1=R[:, 3:4], scalar2=None,
                                                      op0=ALU.is_equal))
                d(lambda e, R=R: e.tensor_scalar(out=R[:, 4:5], in0=R[:, 3:4], scalar1=-1.0, scalar2=None, op0=ALU.mult))
                ph.op("act", lambda e, L=L, R=R: e.activation(out=R[:, 24:28], in_=L[:, 0:4], func=AF.Exp, bias=R[:, 4:5],
                                                             accum_out=R[:, 5:6]), r=[rk, lgk], w=[rk])
                d(lambda e, R=R: e.tensor_scalar(out=R[:, 20:24], in0=R[:, 16:20], scalar1=-1.0, scalar2=1e9,
                                                 op0=ALU.add, op1=ALU.mult))
                for g in range(4):
                    d(lambda e, L=L, R=R, g=g: e.tensor_scalar(
                        out=R[:, 32 + g * 8:40 + g * 8], in0=L[:, 4 + g * 8:12 + g * 8], scalar1=R[:, 20 + g:21 + g],
                        scalar2=None, op0=ALU.add))
                d(lambda e, R=R: e.tensor_reduce(out=R[:, 6:7], in_=R[:, 32:64], axis=AX.X, op=ALU.max))
                d(lambda e, R=R: e.tensor_scalar(out=R[:, 64:96], in0=R[:, 32:64], scalar1=R[:, 6:7], scalar2=None,
                                                 op0=ALU.is_equal))
                d(lambda e, R=R: e.scalar_tensor_tensor(out=R[:, 32:64], in0=R[:, 64:96], scalar=-1e9, in1=R[:, 32:64],
                                                        op0=ALU.mult, op1=ALU.add))
                d(lambda e, R=R: e.tensor_reduce(out=R[:, 7:8], in_=R[:, 32:64], axis=AX.X, op=ALU.max))
                d(lambda e, R=R: e.tensor_scalar(out=R[:, 96:128], in0=R[:, 32:64], scalar1=R[:, 7:8], scalar2=None,
                                                 op0=ALU.is_equal))
                d(lambda e, R=R: e.tensor_tensor(out=R[:, 8:9], in0=R[:, 7:8], in1=R[:, 6:7], op=ALU.subtract))
                ph.op("act", lambda e, R=R: e.activation(out=R[:, 9:10], in_=R[:, 8:9], func=AF.Exp), r=[rk], w=[rk])
                d(lambda e, R=R: e.tensor_scalar(out=R[:, 10:11], in0=R[:, 9:10], scalar1=1.0, scalar2=R[:, 5:6],
                                                 op0=ALU.add, op1=ALU.mult))
                d(lambda e, R=R: e.reciprocal(out=R[:, 11:12], in_=R[:, 10:11]))
                d(lambda e, R=R: e.tensor_tensor(out=R[:, 12:13], in0=R[:, 11:12], in1=R[:, 9:10], op=ALU.mult))
                d(lambda e, R=R, i=i: e.tensor_copy(out=gates_all[:, i, :], in_=R[:, 11:13]), w_=["gates"])
                A_, Ak = asum[j2], ("asum", j2)
                d(lambda e, R=R, A_=A_: e.tensor_tensor(out=A_[:, :], in0=R[:, 64:96], in1=R[:, 96:128], op=ALU.add), w_=[Ak])
                pc, pck = prr.next()

                def cmm(e, pc=pc, A_=A_):
                    e.matmul(pc[:, :32], lhsT=cs[:, C_TS:C_TS + 128], rhs=A_[:, :], start=True, stop=False)
                    return e.matmul(pc[:, :32], lhsT=cs[:, C_OP:C_OP + 128], rhs=ACCA[:, :], start=False, stop=True)
                ph.op("pe", cmm, r=[Ak, "ACCA", "cs"], w=[pck])
                ph.op("pool", lambda e, A_=A_: e.tensor_tensor(out=ACCA[:, :], in0=ACCA[:, :], in1=A_[:, :], op=ALU.add),
                      r=[Ak, "ACCA"], w=["ACCA"])
                C_, Ck = cum[j2], ("cum", j2)
                ph.op("dve", lambda e, pc=pc, C_=C_: e.tensor_tensor(out=C_[:, 0:32], in0=pc[:, :32], in1=iocap[:, :],
                                                                    op=ALU.add), r=[pck, "iocap"], w=[Ck])
                for k, oc in ((0, 64), (1, 96)):
                    d(lambda e, C_=C_, R=R, oc=oc, k=k: e.tensor_tensor(out=C_[:, 32 + 32 * k:64 + 32 * k], in0=C_[:, 0:32],
                                                                       in1=R[:, oc:oc + 32], op=ALU.mult), r_=[Ck], w_=[Ck])
                    d(lambda e, C_=C_, R=R, k=k: e.tensor_reduce(out=R[:, 13 + k:14 + k], in_=C_[:, 32 + 32 * k:64 + 32 * k],
                                                                axis=AX.X, op=ALU.add), r_=[Ck])
                slk = ("sli", j2)
                d(lambda e, R=R, j2=j2: e.tensor_scalar(out=sli[j2][:, :], in0=R[:, 13:15], scalar1=0.25, scalar2=None,
                                                       op0=ALU.add), w_=[slk])
                ph.op("pool", lambda e, j2=j2, i=i: e.tensor_copy(out=slots_all[:, i, :], in_=sli[j2][:, :]),
                      r=[slk], w=["slots"])
                for k in range(2):
                    ph.op("pool", lambda e, j2=j2, k=k: e.indirect_dma_start(
                        out=XS_d[:, :], out_offset=bass.IndirectOffsetOnAxis(ap=sli[j2][:, k:k + 1], axis=0),
                        in_=u2b[j2][:, :], in_offset=None),
                        r=[slk, ("u2b", j2)], w=[], dma=True)
            for s in range(3):
                do_tile(s)
        for b in range(NQB):
            do_block(b)

    if stop_after < 5:
        outer.close()
        return nc

    with Phase(nc, "p5") as ph:
        cs = ph.sb("cs", [128, 128], F32)
        identb = ph.sb("identb", [128, 128], BF16)
        wst = [ph.sb(f"wst{i}", [128, 8, 512], F32) for i in range(6)]
        w1b = [ph.sb(f"w1b{i}", [128, 8, 512], BF16) for i in range(2)]
        w3b = [ph.sb(f"w3b{i}", [128, 8, 512], BF16) for i in range(2)]
        w2b = [ph.sb(f"w2b{i}", [128, 4, D], BF16) for i in range(2)]
        xs = [ph.sb(f"xs{i}", [128, 3, D], BF16) for i in range(2)]
        xsT = [ph.sb(f"xsT{i}", [128, 8, CAP], BF16) for i in range(2)]
        sil = [ph.sb(f"sil{i}", [128, CAP], F32) for i in range(2)]
        hid = [ph.sb(f"hid{i}", [128, 4, CAP], BF16) for i in range(2)]
        yt = [ph.sb(f"yt{i}", [128, D], F32) for i in range(2)]
        pT = [ph.ps(f"pT{i}", [128, 1024], BF16) for i in range(2)]
        pH = [ph.ps(f"pH{i}", [128, 512]) for i in range(4)]
        pY = [ph.ps(f"pY{i}", [128, 512]) for i in range(2)]
        ph.dma(cs[:, :], cst[:, C_ID:C_ID + 128], w=["cs"])
        ph.op("pool", lambda e: e.tensor_copy(out=identb[:, :], in_=cs[:, :]), r=["cs"], w=["identb"])
        wsr, pTr, pHr, pYr, silr, ytr = Ring("wst", wst), Ring("pT", pT), Ring("pH", pH), Ring("pY", pY), Ring("sil", sil), Ring("yt", yt)

        def mmn(e, ps_ap, n, lhs_fn, rhs_fn):
            ins = None
            for c in range(n):
                ins = e.matmul(ps_ap, lhsT=lhs_fn(c), rhs=rhs_fn(c), start=(c == 0), stop=(c == n - 1))
            return ins

        def part1(ex):
            i2 = ex % 2
            ws, wk = wsr.next()
            ph.dma(ws[:, :, :], w1[ex].rearrange("(c p) n -> p c n", p=128), w=[wk])
            ph.op("act", lambda e, ws=ws, i2=i2: e.copy(out=w1b[i2][:, :, :], in_=ws[:, :, :]), r=[wk], w=[("w1b", i2)])
            ws, wk = wsr.next()
            ph.dma(ws[:, :, :], w3[ex].rearrange("(c p) n -> p c n", p=128), w=[wk], eng="act")
            ph.op("act", lambda e, ws=ws, i2=i2: e.copy(out=w3b[i2][:, :, :], in_=ws[:, :, :]), r=[wk], w=[("w3b", i2)])
            ws, wk = wsr.next()
            wsv = ws[:, :, :].rearrange("p c n -> p (c n)").rearrange("p (c n) -> p c n", c=4)
            ph.dma(wsv, w2[ex].rearrange("(c p) n -> p c n", p=128), w=[wk], eng="act")
            ph.op("dve", lambda e, wsv=wsv, i2=i2: e.tensor_copy(out=w2b[i2][:, :, :], in_=wsv), r=[wk], w=[("w2b", i2)])
            ph.dma(xs[i2][:, :, :], XS_d[ex * CAP:(ex + 1) * CAP, :].rearrange("(s p) d -> p s d", p=128), w=[("xs", i2)])
            for s in range(3):
                p_, pk = pTr.next()

                def tr(e, p_=p_, s=s, i2=i2):
                    ins = None
                    for c in range(8):
                        ins = e.transpose(out=p_[:, c * 128:(c + 1) * 128], in_=xs[i2][:, s, c * 128:(c + 1) * 128],
                                          identity=identb[:, :])
                    return ins
                ph.op("pe", tr, r=[("xs", i2), "identb"], w=[pk])
                ph.op("act", lambda e, p_=p_, s=s, i2=i2: e.copy(
                    out=xsT[i2][:, :, s * 128:(s + 1) * 128], in_=p_[:, :].rearrange("p (c t) -> p c t", c=8)),
                    r=[pk], w=[("xsT", i2, s)])
            xk = [("xsT", i2, s) for s in range(3)]
            for f in range(4):
                fs = slice(f * 128, (f + 1) * 128)
                pa, pak = pHr.next()
                pb, pbk = pHr.next()
                sl, slk = silr.next()
                ph.op("pe", lambda e, pa=pa, fs=fs, i2=i2: mmn(e, pa[:, :CAP], 8, lambda c: w1b[i2][:, c, fs],
                                                              lambda c: xsT[i2][:, c, :]), r=[("w1b", i2)] + xk, w=[pak])
                ph.op("pe", lambda e, pb=pb, fs=fs, i2=i2: mmn(e, pb[:, :CAP], 8, lambda c: w3b[i2][:, c, fs],
                                                              lambda c: xsT[i2][:, c, :]), r=[("w3b", i2)] + xk, w=[pbk])
                ph.op("act", lambda e, pa=pa, sl=sl: e.activation(out=sl[:, :], in_=pa[:, :CAP], func=AF.Silu), r=[pak], w=[slk])
                ph.op("dve", lambda e, pb=pb, sl=sl, f=f, i2=i2: e.tensor_tensor(
                    out=hid[i2][:, f, :], in0=pb[:, :CAP], in1=sl[:, :], op=ALU.mult), r=[pbk, slk], w=[("hid", i2, f)])

        def part2(ex):
            i2 = ex % 2
            hk = [("hid", i2, f) for f in range(4)]
            for s in range(3):
                y_, yk = ytr.next()
                for half in range(2):
                    py, pyk = pYr.next()
                    ph.op("pe", lambda e, py=py, s=s, half=half, i2=i2: mmn(
                        e, py[:, :], 4, lambda c: hid[i2][:, c, s * 128:(s + 1) * 128],
                        lambda c: w2b[i2][:, c, half * 512:(half + 1) * 512]), r=hk + [("w2b", i2)], w=[pyk])
                    ph.op("act", lambda e, py=py, y_=y_, half=half: e.copy(out=y_[:, half * 512:(half + 1) * 512], in_=py[:, :]),
                          r=[pyk], w=[yk + (half,)])
                ph.dma(YS_d[ex * CAP + s * 128:ex * CAP + (s + 1) * 128, :], y_[:, :], r=[yk + (0,), yk + (1,)])

        for ex in range(NEXP + 1):
            if ex < NEXP:
                part1(ex)
            if ex >= 1:
                part2(ex - 1)

    with Phase(nc, "p6") as ph:
        gFB = ph.sb("gFB", [128, D], F32)
        y1 = [ph.sb(f"y1_{i}", [128, D], F32) for i in range(4)]
        y2 = [ph.sb(f"y2_{i}", [128, D], F32) for i in range(4)]
        h1t = [ph.sb(f"h1t{i}", [128, D], F32) for i in range(4)]
        acc = [ph.sb(f"acc{i}", [128, D], F32) for i in range(4)]
        ot = [ph.sb(f"ot{i}", [128, D], F32) for i in range(4)]
        junk = ph.sb("junk", [128, D], BF16)
        st = ph.sb("st", [128, NT, 4], F32)
        ph.dma(gFB[:, :], gF[0:1, :].partition_broadcast(128), w=["gFB"])
        for i in range(NT):
            j2 = i % 4
            for k, yy in ((0, y1), (1, y2)):
                ph.op("pool", lambda e, yy=yy, j2=j2, i=i, k=k: e.indirect_dma_start(
                    out=yy[j2][:, :], out_offset=None, in_=YS_d[:, :],
                    in_offset=bass.IndirectOffsetOnAxis(ap=slots_all[:, i, k:k + 1], axis=0)),
                    r=[], w=[("y", k, j2)], dma=True)
            ph.dma(h1t[j2][:, :], H1_d[i * 128:(i + 1) * 128, :], w=[("h1t", j2)])
            ph.op("dve", lambda e, j2=j2, i=i: e.scalar_tensor_tensor(
                out=acc[j2][:, :], in0=y1[j2][:, :], scalar=gates_all[:, i, 0:1], in1=h1t[j2][:, :], op0=ALU.mult, op1=ALU.add),
                r=[("y", 0, j2), ("h1t", j2)], w=[("acc", j2)])
            ph.op("dve", lambda e, j2=j2, i=i: e.scalar_tensor_tensor(
                out=acc[j2][:, :], in0=y2[j2][:, :], scalar=gates_all[:, i, 1:2], in1=acc[j2][:, :], op0=ALU.mult, op1=ALU.add),
                r=[("y", 1, j2), ("acc", j2)], w=[("acc", j2)])
            ph.op("act", lambda e, j2=j2, i=i: e.activation(out=junk[:, :], in_=acc[j2][:, :], func=AF.Square,
                                                           accum_out=st[:, i, 0:1]), r=[("acc", j2)], w=["junk", ("st", i)])
            ph.op("act", lambda e, i=i: e.activation(out=st[:, i, 1:2], in_=st[:, i, 0:1], func=AF.Sqrt, bias=EPS, scale=1.0 / D),
                  r=[("st", i)], w=[("st", i)])
            ph.op("dve", lambda e, i=i: e.reciprocal(out=st[:, i, 2:3], in_=st[:, i, 1:2]), r=[("st", i)], w=[("st", i)])
            ph.op("dve", lambda e, j2=j2, i=i: e.scalar_tensor_tensor(
                out=ot[j2][:, :], in0=acc[j2][:, :], scalar=st[:, i, 2:3], in1=gFB[:, :], op0=ALU.mult, op1=ALU.mult),
                r=[("acc", j2), ("st", i), "gFB"], w=[("ot", j2)])
            lo_t = max(i * 128, NMETA)
            hi_t = min((i + 1) * 128, NMETA + SEQ)
            ph.dma(out[lo_t - NMETA:hi_t - NMETA, :], ot[j2][lo_t - i * 128:hi_t - i * 128, :], r=[("ot", j2)])

    outer.close()
    return nc


def _sel_cols(w, idx):
    idx = np.asarray(idx)
    o = w[:, np.maximum(idx, 0)].copy()
    o[:, idx < 0] = 0.0
    return o


def _winB_index():
    idx = -np.ones(NB, dtype=np.int64)
    for p in range(4):
        for hh in range(2):
            h = 2 * p + hh
            for n in range(48):
                idx[B_QBN + p * 128 + hh * 64 + n] = O_QB + h * 64 + 16 + n
    for h in range(8):
        for r in range(16):
            idx[B_QBR + h * 32 + r] = O_QB + h * 64 + r
            idx[B_QBRT + h * 32 + r] = O_QB + h * 64 + (r + 8) % 16
    for g in range(4):
        for r in range(16):
            idx[B_KR + g * 32 + r] = O_KR + r
            idx[B_KRT + g * 32 + r] = O_KR + (r + 8) % 16
    for g in range(2):
        for d in range(64):
            idx[B_KI + g * 64 + d] = O_KI + d
            if d < 16:
                idx[B_KIT + g * 64 + d] = O_KI + (d + 8) % 16
    for half, (bm, bt) in enumerate(((B_QI0, B_QI0T), (B_QI1, B_QI1T))):
        for j in range(256):
            col = half * 256 + j
            h, d = col // 64, col % 64
            idx[bm + j] = O_QI + col
            if d < 16:
                idx[bt + j] = O_QI + h * 64 + (d + 8) % 16
    for c in range(128):
        idx[B_CKV + c] = O_CKV + c
    for h in range(8):
        idx[B_WI + h] = O_WI + h
    for j in range(1024):
        idx[B_GA + j] = O_GA + j
        idx[B_GB + j] = O_GB + j
    return idx


def _tables():
    half = 8
    inv = (np.float32(500000.0) ** (-np.arange(half, dtype=np.float32) / np.float32(half))).astype(np.float32)
    pos = np.arange(T, dtype=np.float32)
    ang = (pos[:, None] * inv[None, :]).astype(np.float32)
    cos = np.cos(ang).astype(np.float32).T
    sin = np.sin(ang).astype(np.float32).T
    tabs = np.zeros((4, 128, T), np.float32)
    for ts, period in ((0, 32), (1, 64)):
        for r in range(128):
            rr = r % period
            if rr < 16:
                j = rr % 8
                tabs[2 * ts, r] = cos[j]
                tabs[2 * ts + 1, r] = (-sin[j]) if rr < 8 else sin[j]
            else:
                tabs[2 * ts, r] = 1.0
    return tabs


def _consts():
    c = np.zeros((128, NCST), np.float32)
    k = np.arange(128)
    c[:, C_ID:C_ID + 128] = np.eye(128, dtype=np.float32)
    c[:, C_TN:C_TN + 128] = -(k[:, None] >= k[None, :]).astype(np.float32)
    c[:, C_ON:C_ON + 128] = -1.0
    c[:, C_OP:C_OP + 128] = 1.0
    c[:, C_TS:C_TS + 128] = (k[:, None] < k[None, :]).astype(np.float32)
    q = np.arange(QB)
    for m in range(3):
        c[:, C_SBM + m * QB:C_SBM + (m + 1) * QB] = ((128 * m + k)[:, None] < q[None, :]).astype(np.float32)
    g = np.where(k < 16, 0, np.where(k < 80, 1, 2))
    NEG = -1e30
    c[:, C_MD:C_MD + 128] = np.where(g[None, :] <= g[:, None], 0.0, NEG)
    c[:, C_MN:C_MN + 128] = np.where((k[None, :] < 16) & (k[:, None] >= 80), 0.0, NEG)
    c[:, C_IO:C_IO + 32] = np.arange(32, dtype=np.float32)[None, :]
    for i in range(NITER + 1):
        c[:, C_P2 + i] = 2.0 ** (-(i + 1))
    return c


_CACHE = {}


def _host_inputs(inputs):
    f = lambda a: np.ascontiguousarray(np.asarray(a, dtype=np.float32))
    w_in = f(inputs["w_in"])[0]
    shared = {
        "g1": f(inputs["norm_mix_g"])[0:1],
        "g2": f(inputs["norm_ffn_g"])[0:1],
        "gF": f(inputs["norm_final_g"]).reshape(1, D),
        "winA": np.ascontiguousarray(w_in[:, 0:1536]),
        "winB": np.ascontiguousarray(_sel_cols(w_in, _winB_index())),
        "tabs": _tables(),
        "cst": _consts(),
        "wupa": f(inputs["w_up_a"])[0],
        "wupb": f(inputs["w_up_b"])[0],
        "wo": f(inputs["w_o"])[0],
        "wr": np.ascontiguousarray(np.concatenate([f(inputs["w_group"])[0], f(inputs["w_router"])[0]], axis=1)),
        "br": np.ascontiguousarray(np.concatenate([f(inputs["b_group"])[0], f(inputs["b_router"])[0]])[None, :]),
        "w1": f(inputs["w1"])[0],
        "w3": f(inputs["w3"])[0],
        "w2": f(inputs["w2"])[0],
    }
    wuk = f(inputs["w_uk"])[0]
    wukT = np.zeros((128, 4, 128), np.float32)
    for h in range(8):
        wukT[(h % 2) * 64:(h % 2) * 64 + 48, h // 2, :] = wuk[h].T
    shared["wukT"] = wukT
    shared["wuv"] = np.ascontiguousarray(np.transpose(f(inputs["w_uv"])[0], (1, 0, 2)))
    x = f(inputs["x"])
    meta = f(inputs["meta_tokens"])
    maps = []
    for b in range(NCORES):
        h0 = np.zeros((T, D), np.float32)
        h0[:NMETA] = meta
        h0[NMETA:NMETA + SEQ] = x[b]
        m = dict(shared)
        m["h0"] = h0
        maps.append(m)
    return maps


def kernel(**inputs):
    if "nc" not in _CACHE:
        _CACHE["nc"] = build_program()
    nc = _CACHE["nc"]
    maps = _host_inputs(inputs)
    res = run_bass_kernel_spmd(nc, maps, core_ids=list(range(NCORES)))
    return np.stack([np.asarray(r["out"], dtype=np.float32) for r in res.results], axis=0)
```

```python
import numpy as np
from contextlib import ExitStack
import concourse.bass as bass
import concourse.mybir as mybir
from concourse.bass_utils import run_bass_kernel_spmd

F32 = mybir.dt.float32
BF16 = mybir.dt.bfloat16
I32 = mybir.dt.int32
AF = mybir.ActivationFunctionType
ALU = mybir.AluOpType
AX = mybir.AxisListType

NCORES = 8
SEQ = 4096
NMETA = 16
T = 4224
NT = T // 128
D = 1024
EPS = 1e-6
QB = 384
NQB = T // QB
CAP = 384
NEXP = 32
NSLOT = NEXP * CAP
NITER = 16
TOPK = 256

O_QA, O_KA, O_VA, O_QB, O_CKV, O_KR, O_QI, O_KI, O_WI, O_GA, O_GB = (
    0, 512, 1024, 1536, 2048, 2176, 2192, 2704, 2768, 2776, 3800)

B_QBN, B_QBR, B_QBRT, B_KR, B_KRT, B_KI, B_KIT, B_QI0, B_QI0T, B_QI1, B_QI1T, B_CKV, B_WI, B_GA, B_GB = (
    0, 512, 768, 1024, 1152, 1280, 1408, 1536, 1792, 2048, 2304, 2560, 2688, 2696, 3720)
NB = 4744

C_ID, C_TN, C_ON, C_OP, C_TS, C_SBM, C_MD, C_MN, C_IO, C_P2 = (
    0, 128, 256, 384, 512, 640, 640 + 1152, 640 + 1152 + 128, 640 + 1152 + 256, 640 + 1152 + 256 + 32)
NCST = C_P2 + 2 * NITER + 2


class Op:
    __slots__ = ("eng", "fn", "dma", "deps", "signals", "sem", "val", "didx")

    def __init__(self, eng, fn, dma):
        self.eng = eng
        self.fn = fn
        self.dma = dma
        self.deps = []
        self.signals = False
        self.sem = None
        self.val = 0
        self.didx = -1


class Phase:
    K = 6
    ENGS = ("pe", "act", "dve", "pool", "sp")

    def __init__(self, nc, name):
        self.nc = nc
        self.name = name
        self.es = ExitStack()
        self.ops = {e: [] for e in self.ENGS}
        self.lastw = {}
        self.rd_c = {}
        self.rd_d = {}

    def __enter__(self):
        self.es.__enter__()
        return self

    def __exit__(self, *a):
        if a[0] is None:
            self.emit()
        return self.es.__exit__(*a)

    def sb(self, name, shape, dt):
        return self.es.enter_context(self.nc.sbuf_tensor(f"{self.name}_{name}", shape, dt))

    def ps(self, name, shape, dt=F32):
        return self.es.enter_context(self.nc.psum_tensor(f"{self.name}_{name}", shape, dt))

    def _dep(self, o, d, raw):
        if d is o:
            return
        if (not o.dma) and (not d.dma) and o.eng == d.eng:
            if not raw or o.eng == "pe":
                return
        if d not in o.deps:
            o.deps.append(d)
            d.signals = True

    def op(self, eng, fn, r=(), w=(), dma=False):
        o = Op(eng, fn, dma)
        for k in r:
            lw = self.lastw.get(k)
            if lw is not None:
                self._dep(o, lw, True)
        for k in w:
            lw = self.lastw.get(k)
            if lw is not None:
                self._dep(o, lw, False)
            for d in self.rd_c.get(k, {}).values():
                self._dep(o, d, False)
            for d in self.rd_d.get(k, ()):
                self._dep(o, d, False)
        for k in r:
            if dma:
                self.rd_d.setdefault(k, []).append(o)
            else:
                self.rd_c.setdefault(k, {})[eng] = o
        for k in w:
            self.lastw[k] = o
            self.rd_c[k] = {}
            self.rd_d[k] = []
        self.ops[eng].append(o)
        return o

    def dma(self, out, in_, r=(), w=(), eng="sp", **kw):
        return self.op(eng, lambda e: e.dma_start(out=out, in_=in_, **kw), r=r, w=w, dma=True)

    def emit(self):
        nc = self.nc
        es = self.es
        K = self.K
        dsems = {}
        dlist = {}
        for e in self.ENGS:
            c = 0
            dl = []
            csem = None
            for o in self.ops[e]:
                if o.dma:
                    n = len(dl)
                    if e not in dsems:
                        dsems[e] = [es.enter_context(nc.semaphore(f"{self.name}_d{e}{i}")) for i in range(K)]
                    o.didx = n
                    o.sem = (f"d{e}{n % K}", dsems[e][n % K])
                    o.val = 16 * (n // K + 1)
                    dl.append(o)
                elif o.signals:
                    if csem is None:
                        csem = es.enter_context(nc.semaphore(f"{self.name}_c{e}"))
                    c += 1
                    o.sem = (f"c{e}", csem)
                    o.val = c
            dlist[e] = dl
        hmap = {"pe": "tensor", "act": "scalar", "dve": "vector", "pool": "gpsimd", "sp": "sync"}
        with nc.Block() as block:
            for e in self.ENGS:
                if not self.ops[e]:
                    continue

                def body(eng, e=e):
                    waited = {}
                    for o in self.ops[e]:
                        waits = {}
                        for d in o.deps:
                            nm, s = d.sem
                            if waits.get(nm, (0, None))[0] < d.val:
                                waits[nm] = (d.val, s)
                        if o.dma and o.didx >= K:
                            p = dlist[e][o.didx - K]
                            nm, s = p.sem
                            if waits.get(nm, (0, None))[0] < p.val:
                                waits[nm] = (p.val, s)
                        for nm, (v, s) in waits.items():
                            if waited.get(nm, 0) < v:
                                eng.wait_ge(s, v)
                                waited[nm] = v
                        ins = o.fn(eng)
                        if o.dma:
                            ins.then_inc(o.sem[1], 16)
                        elif o.signals:
                            ins.then_inc(o.sem[1], 1)
                    fin = {}
                    for o in dlist[e]:
                        fin[o.sem[0]] = (o.val, o.sem[1])
                    for nm, (v, s) in fin.items():
                        if waited.get(nm, 0) < v:
                            eng.wait_ge(s, v)

                getattr(block, hmap[e])(body)


class Ring:
    def __init__(self, name, bufs):
        self.name = name
        self.bufs = bufs
        self.i = -1

    def next(self):
        self.i += 1
        j = self.i % len(self.bufs)
        return self.bufs[j], (self.name, j)


def build_program(debug_out=(), stop_after=99):
    nc = bass.Bass("TRN2", target_bir_lowering=False)

    def din(name, shape, dt=F32):
        return nc.dram_tensor(name, list(shape), dt, kind="ExternalInput")

    def dscr(name, shape, dt):
        kind = "ExternalOutput" if name in debug_out else "Internal"
        return nc.dram_tensor(name, list(shape), dt, kind=kind)

    h0 = din("h0", [T, D])
    g1 = din("g1", [1, D])
    g2 = din("g2", [1, D])
    gF = din("gF", [1, D])
    winA = din("winA", [D, 1536])
    winB = din("winB", [D, NB])
    tabs = din("tabs", [4, 128, T])
    cst = din("cst", [128, NCST])
    wukT = din("wukT", [128, 4, 128])
    wuv = din("wuv", [128, 8, 64])
    wupa = din("wupa", [512, D])
    wupb = din("wupb", [512, D])
    wo = din("wo", [D, D])
    wr = din("wr", [D, 36])
    br = din("br", [1, 36])
    if stop_after >= 5:
        w1 = din("w1", [NEXP, D, 512])
        w3 = din("w3", [NEXP, D, 512])
        w2 = din("w2", [NEXP, 512, D])
    out = nc.dram_tensor("out", [SEQ, D], F32, kind="ExternalOutput")

    unT_d = dscr("unT_d", [128, 8, T], BF16)
    QA_d = dscr("QA_d", [512, T], BF16)
    KA_d = dscr("KA_d", [512, T], BF16)
    VA_d = dscr("VA_d", [T, 512], BF16)
    QBN_d = dscr("QBN_d", [512, T], BF16)
    QBR_d = dscr("QBR_d", [256, T], BF16)
    KR_d = dscr("KR_d", [128, T], BF16)
    KI_d = dscr("KI_d", [128, T], BF16)
    QI_d = dscr("QI_d", [512, T], BF16)
    CKVT_d = dscr("CKVT_d", [128, T], BF16)
    CKV_d = dscr("CKV_d", [T, 128], BF16)
    WI_d = dscr("WI_d", [T, 8], F32)
    YA_d = dscr("YA_d", [512, T], BF16)
    YB_d = dscr("YB_d", [512, T], BF16)
    H1_d = dscr("H1_d", [T, D], F32)
    XS_d = dscr("XS_d", [NSLOT + 128, D], BF16)
    YS_d = dscr("YS_d", [NSLOT + 128, D], F32)

    blocks512 = [(i * 512, 512) for i in range(8)] + [(4096, 128)]
    outer = ExitStack()
    slots_all = outer.enter_context(nc.sbuf_tensor("slots_all", [128, NT, 2], I32))
    gates_all = outer.enter_context(nc.sbuf_tensor("gates_all", [128, NT, 2], F32))

    with Phase(nc, "p1") as ph:
        unT = ph.sb("unT", [128, 8, T], BF16)
        gB = ph.sb("gB", [128, D], F32)
        cstf = ph.sb("cstf", [128, 128], F32)
        identb = ph.sb("identb", [128, 128], BF16)
        xt = [ph.sb(f"xt{i}", [128, D], F32) for i in range(2)]
        xn = [ph.sb(f"xn{i}", [128, D], BF16) for i in range(2)]
        junk = ph.sb("junk", [128, D], BF16)
        ss = ph.sb("ss", [128, NT], F32)
        rt = ph.sb("rt", [128, NT], F32)
        rstd = ph.sb("rstd", [128, NT], F32)
        wst = [ph.sb(f"wst{i}", [128, 8, 512], F32) for i in range(2)]
        wbf = [ph.sb(f"wbf{i}", [128, 8, 512], BF16) for i in range(2)]
        tb = [ph.sb(f"tb{i}", [128, 2, 512], F32) for i in range(3)]
        t1 = [ph.sb(f"t1_{i}", [128, 512], F32) for i in range(2)]
        t2 = [ph.sb(f"t2_{i}", [128, 512], F32) for i in range(2)]
        ost = [ph.sb(f"ost{i}", [128, 512], BF16) for i in range(4)]
        osf = [ph.sb(f"osf{i}", [128, 8], F32) for i in range(2)]
        pT = [ph.ps(f"pT{i}", [128, 1024], BF16) for i in range(2)]
        pp = [ph.ps(f"pp{i}", [128, 512], F32) for i in range(4)]

        ph.dma(gB[:, :], g1[0:1, :].partition_broadcast(128), w=["gB"])
        ph.dma(cstf[:, :], cst[:, C_ID:C_ID + 128], w=["cstf"])
        ph.op("pool", lambda e: e.tensor_copy(out=identb[:, :], in_=cstf[:, :]), r=["cstf"], w=["identb"])

        for i in range(NT):
            x_, xk = xt[i % 2], ("xt", i % 2)
            n_, nk = xn[i % 2], ("xn", i % 2)
            p_, pk = pT[i % 2], ("pT", i % 2)
            ph.dma(x_[:, :], h0[i * 128:(i + 1) * 128, :], w=[xk])
            ph.op("act", lambda e, x_=x_, i=i: e.activation(out=junk[:, :], in_=x_[:, :], func=AF.Square,
                                                          accum_out=ss[:, i:i + 1]),
                  r=[xk], w=["junk", ("ss", i)])
            ph.op("act", lambda e, i=i: e.activation(out=rt[:, i:i + 1], in_=ss[:, i:i + 1], func=AF.Sqrt,
                                                    bias=EPS, scale=1.0 / D),
                  r=[("ss", i)], w=[("rt", i)])
            ph.op("dve", lambda e, i=i: e.reciprocal(out=rstd[:, i:i + 1], in_=rt[:, i:i + 1]),
                  r=[("rt", i)], w=[("rstd", i)])
            ph.op("dve", lambda e, x_=x_, n_=n_, i=i: e.scalar_tensor_tensor(
                out=n_[:, :], in0=x_[:, :], scalar=rstd[:, i:i + 1], in1=gB[:, :], op0=ALU.mult, op1=ALU.mult),
                r=[xk, ("rstd", i), "gB"], w=[nk])

            def tr(e, n_=n_, p_=p_):
                ins = None
                for c in range(8):
                    ins = e.transpose(out=p_[:, c * 128:(c + 1) * 128], in_=n_[:, c * 128:(c + 1) * 128],
                                      identity=identb[:, :])
                return ins
            ph.op("pe", tr, r=[nk, "identb"], w=[pk])
            ph.op("act", lambda e, p_=p_, i=i: e.copy(
                out=unT[:, :, i * 128:(i + 1) * 128], in_=p_[:, :].rearrange("p (c t) -> p c t", c=8)),
                r=[pk], w=[("unT", i)])
        allun = [("unT", i) for i in range(NT)]
        for c in range(8):
            ph.dma(unT_d[:, c, :], unT[:, c, :], r=allun)

        wring = Ring("w", list(zip(wst, wbf)))
        ppr = Ring("pp", pp)
        ostr = Ring("ost", ost)
        tbr = Ring("tb", tb)
        t1r = Ring("t1", t1)
        t2r = Ring("t2", t2)
        osfr = Ring("osf", osf)

        def load_group(src, c0, n):
            (ws, wb), wk = wring.next()
            ph.dma(ws[:, :, :n], src[:, c0:c0 + n].rearrange("(c p) n -> p c n", p=128), w=[("wst",) + wk])
            ph.op("act", lambda e: e.copy(out=wb[:, :, :n], in_=ws[:, :, :n]),
                  r=[("wst",) + wk], w=[("wbf",) + wk])
            return wb, ("wbf",) + wk

        def mm8(e, ps_ap, lhs_fn, rhs_fn):
            ins = None
            for c in range(8):
                ins = e.matmul(ps_ap, lhsT=lhs_fn(c), rhs=rhs_fn(c), start=(c == 0), stop=(c == 7))
            return ins

        def fm_jobs(src, c0, n, jobs):
            wb, wk = load_group(src, c0, n)
            for (t0, w) in blocks512:
                tiles = [("unT", t0 // 128 + j) for j in range(w // 128)]
                for (lc, dest, drow, mode, arg) in jobs:
                    o_, ok = ostr.next()
                    if mode == "plain":
                        p_, pk = ppr.next()
                        ph.op("pe", lambda e, p_=p_, lc=lc, t0=t0, w=w: mm8(
                            e, p_[:, :w], lambda c: wb[:, c, lc:lc + 128], lambda c: unT[:, c, t0:t0 + w]),
                            r=[wk] + tiles, w=[pk])
                        ph.op("act", lambda e, p_=p_, o_=o_, w=w, arg=arg: e.activation(
                            out=o_[:, :w], in_=p_[:, :w], func=AF.Copy, scale=float(arg)), r=[pk], w=[ok])
                    else:
                        tw, ts = arg
                        p_, pk = ppr.next()
                        q_, qk = ppr.next()
                        tb_, tk = tbr.next()
                        a_, ak = t1r.next()
                        b_, bk = t2r.next()
                        ph.dma(tb_[:, :, :w], tabs[2 * ts:2 * ts + 2, :, t0:t0 + w].rearrange("a p t -> p a t"), w=[tk])
                        ph.op("pe", lambda e, p_=p_, lc=lc, t0=t0, w=w: mm8(
                            e, p_[:, :w], lambda c: wb[:, c, lc:lc + 128], lambda c: unT[:, c, t0:t0 + w]),
                            r=[wk] + tiles, w=[pk])
                        ph.op("pe", lambda e, q_=q_, tw=tw, t0=t0, w=w: mm8(
                            e, q_[:, :w], lambda c: wb[:, c, tw:tw + 128], lambda c: unT[:, c, t0:t0 + w]),
                            r=[wk] + tiles, w=[qk])
                        ph.op("dve", lambda e, a_=a_, p_=p_, tb_=tb_, w=w: e.tensor_tensor(
                            out=a_[:, :w], in0=p_[:, :w], in1=tb_[:, 0, :w], op=ALU.mult), r=[pk, tk], w=[ak])
                        ph.op("dve", lambda e, b_=b_, q_=q_, tb_=tb_, w=w: e.tensor_tensor(
                            out=b_[:, :w], in0=q_[:, :w], in1=tb_[:, 1, :w], op=ALU.mult), r=[qk, tk], w=[bk])
                        ph.op("pool", lambda e, a_=a_, b_=b_, o_=o_, w=w: e.tensor_tensor(
                            out=o_[:, :w], in0=a_[:, :w], in1=b_[:, :w], op=ALU.add), r=[ak, bk], w=[ok])
                    ph.dma(dest[drow:drow + 128, t0:t0 + w], o_[:, :w], r=[ok])

        fm_jobs(winA, O_QA, 512, [(m * 128, QA_d, m * 128, "plain", 0.125) for m in range(4)])
        fm_jobs(winA, O_KA, 512, [(m * 128, KA_d, m * 128, "plain", 1.0) for m in range(4)])
        fm_jobs(winB, B_QBN, 512, [(m * 128, QBN_d, m * 128, "plain", 1.0) for m in range(4)])
        fm_jobs(winB, B_QBR, 512, [(m * 128, QBR_d, m * 128, "rope", (256 + m * 128, 0)) for m in range(2)])
        fm_jobs(winB, B_KR, 512, [(0, KR_d, 0, "rope", (128, 0)), (256, KI_d, 0, "rope", (384, 1))])
        fm_jobs(winB, B_QI0, 512, [(m * 128, QI_d, m * 128, "rope", (256 + m * 128, 1)) for m in range(2)])
        fm_jobs(winB, B_QI1, 512, [(m * 128, QI_d, 256 + m * 128, "rope", (256 + m * 128, 1)) for m in range(2)])
        fm_jobs(winB, B_CKV, 128, [(0, CKVT_d, 0, "plain", 1.0)])

        wb, wk = load_group(winA, O_VA, 512)
        for i in range(NT):
            p_, pk = ppr.next()
            o_, ok = ostr.next()
            ph.op("pe", lambda e, p_=p_, i=i, wb=wb: mm8(
                e, p_[:, :], lambda c: unT[:, c, i * 128:(i + 1) * 128], lambda c: wb[:, c, 0:512]),
                r=[wk, ("unT", i)], w=[pk])
            ph.op("act", lambda e, p_=p_, o_=o_: e.copy(out=o_[:, :], in_=p_[:, :]), r=[pk], w=[ok])
            ph.dma(VA_d[i * 128:(i + 1) * 128, :], o_[:, :], r=[ok])
        wb, wk = load_group(winB, B_CKV, 136)
        for i in range(NT):
            p_, pk = ppr.next()
            o_, ok = ostr.next()
            f_, fk = osfr.next()
            ph.op("pe", lambda e, p_=p_, i=i, wb=wb: mm8(
                e, p_[:, :136], lambda c: unT[:, c, i * 128:(i + 1) * 128], lambda c: wb[:, c, 0:136]),
                r=[wk, ("unT", i)], w=[pk])
            ph.op("act", lambda e, p_=p_, o_=o_: e.copy(out=o_[:, :128], in_=p_[:, :128]), r=[pk], w=[ok])
            ph.op("act", lambda e, p_=p_, f_=f_: e.copy(out=f_[:, :], in_=p_[:, 128:136]), r=[pk], w=[fk])
            ph.dma(CKV_d[i * 128:(i + 1) * 128, :], o_[:, :128], r=[ok])
            ph.dma(WI_d[i * 128:(i + 1) * 128, :], f_[:, :], r=[fk])


    if stop_after < 2:
        return nc

    with Phase(nc, "p2") as ph:
        cs = ph.sb("cs", [128, C_SBM + 3 * QB], F32)
        TNb = ph.sb("TNb", [128, 128], BF16)
        ONb = ph.sb("ONb", [128, 128], BF16)
        SBMb = ph.sb("SBMb", [128, 3, QB], BF16)
        Vt = ph.sb("Vt", [128, NT, 512], BF16)
        QT = [ph.sb(f"QT{i}", [128, T], BF16) for i in range(2)]
        KT = [ph.sb(f"KT{i}", [128, T], BF16) for i in range(2)]
        Es = [ph.sb(f"Es{i}", [128, QB], F32) for i in range(6)]
        SPs = [ph.sb(f"SP{i}", [128, QB], BF16) for i in range(6)]
        Xs = [ph.sb(f"X{i}", [128, QB], F32) for i in range(3)]
        As = [ph.sb(f"A{i}", [128, QB], BF16) for i in range(4)]
        ACC = [ph.sb(f"ACC{i}", [128, QB], BF16) for i in range(3)]
        yst = [ph.sb(f"yst{i}", [128, QB], BF16) for i in range(2)]
        pS = [ph.ps(f"pS{i}", [128, 512]) for i in range(3)]
        pL = [ph.ps(f"pL{i}", [128, 512]) for i in range(3)]
        pY = [ph.ps(f"pY{i}", [128, 512]) for i in range(2)]
        ph.dma(cs[:, :], cst[:, 0:C_SBM + 3 * QB], w=["cs"])
        ph.op("pool", lambda e: e.tensor_copy(out=TNb[:, :], in_=cs[:, C_TN:C_TN + 128]), r=["cs"], w=["TNb"])
        ph.op("pool", lambda e: e.tensor_copy(out=ONb[:, :], in_=cs[:, C_ON:C_ON + 128]), r=["cs"], w=["ONb"])
        ph.op("pool", lambda e: e.tensor_copy(out=SBMb[:, :, :].rearrange("p m q -> p (m q)"),
                                              in_=cs[:, C_SBM:C_SBM + 3 * QB]), r=["cs"], w=["SBMb"])
        for g in range(3):
            ph.dma(Vt[:, g * 11:(g + 1) * 11, :],
                   VA_d[g * 11 * 128:(g + 1) * 11 * 128, :].rearrange("(i p) n -> p i n", p=128), w=[("Vt", g)])
        vkeys = [("Vt", g) for g in range(3)]
        Er, SPr, Xr, Ar = Ring("E", Es), Ring("SP", SPs), Ring("X", Xs), Ring("A", As)
        pSr, pLr, pYr = Ring("pS", pS), Ring("pL", pL), Ring("pY", pY)
        ACCr, ystr = Ring("ACC", ACC), Ring("yst", yst)

        def load_pair(p):
            ph.dma(QT[p % 2][:, :], QA_d[p * 128:(p + 1) * 128, :], w=[("QT", p % 2)])
            ph.dma(KT[p % 2][:, :], KA_d[p * 128:(p + 1) * 128, :], w=[("KT", p % 2)])

        units = []
        for h in range(8):
            for b in range(NQB):
                grp = {"h": h, "b": b}
                jl = list(range(3 * b + 2, -1, -1))
                for j in jl:
                    units.append({"g": grp, "j": j, "first": j == jl[0], "last": j == 0})

        def stageA(u):
            g = u["g"]
            h, b, j = g["h"], g["b"], u["j"]
            p = h // 2
            if u["first"] and b == 0 and h % 2 == 0:
                if p == 0:
                    load_pair(0)
                if p + 1 < 4:
                    load_pair(p + 1)
            hr = slice((h % 2) * 64, (h % 2) * 64 + 64)
            Q_, K_ = QT[p % 2], KT[p % 2]
            qk, kk = ("QT", p % 2), ("KT", p % 2)
            q0 = b * QB
            m = j - 3 * b
            c0 = 128 * m if m > 0 else 0
            cw = slice(c0, QB)
            s_, sk = pSr.next()
            E_, ek = Er.next()
            SP_, spk = SPr.next()
            u.update(hr=hr, cw=cw, E=E_, ek=ek, SP=SP_, spk=spk)
            ph.op("pe", lambda e: e.matmul(s_[:, cw], lhsT=K_[hr, j * 128:(j + 1) * 128], rhs=Q_[hr, q0 + c0:q0 + QB],
                                           start=True, stop=True), r=[qk, kk], w=[sk])
            ph.op("act", lambda e: e.activation(out=E_[:, cw], in_=s_[:, cw], func=AF.Exp), r=[sk], w=[ek])
            ph.op("act", lambda e: e.activation(out=SP_[:, cw], in_=E_[:, cw], func=AF.Ln, bias=1.0), r=[ek], w=[spk])
            if m >= 0:
                ph.op("pool", lambda e: e.tensor_tensor(out=SP_[:, cw], in0=SP_[:, cw], in1=SBMb[:, m, cw], op=ALU.mult),
                      r=[spk, "SBMb"], w=[spk])
                ph.op("pool", lambda e: e.tensor_tensor(out=E_[:, cw], in0=E_[:, cw], in1=SBMb[:, m, cw], op=ALU.mult),
                      r=[ek, "SBMb"], w=[ek])

        def stageB(u):
            g = u["g"]
            cw, E_, ek, SP_, spk = u["cw"], u["E"], u["ek"], u["SP"], u["spk"]
            if u["first"]:
                acc, acck = ACCr.next()
                g["acc"], g["acck"] = acc, acck
                ph.op("pool", lambda e: e.memset(acc[:, :], 0.0), w=[acck])
            acc, acck = g["acc"], g["acck"]
            l_, lk = pLr.next()
            X_, xk2 = Xr.next()
            A_, ak = Ar.next()
            u.update(A=A_, ak=ak)

            def lmm(e):
                e.matmul(l_[:, cw], lhsT=TNb[:, :], rhs=SP_[:, cw], start=True, stop=False)
                return e.matmul(l_[:, cw], lhsT=ONb[:, :], rhs=acc[:, cw], start=False, stop=True)
            ph.op("pe", lmm, r=[spk, acck, "TNb", "ONb"], w=[lk])
            ph.op("act", lambda e: e.activation(out=X_[:, cw], in_=l_[:, cw], func=AF.Exp), r=[lk], w=[xk2])
            ph.op("dve", lambda e: e.tensor_tensor(out=A_[:, cw], in0=X_[:, cw], in1=E_[:, cw], op=ALU.mult),
                  r=[xk2, ek], w=[ak])
            ph.op("pool", lambda e: e.tensor_tensor(out=acc[:, cw], in0=acc[:, cw], in1=SP_[:, cw], op=ALU.add),
                  r=[acck, spk], w=[acck])

        def stageC(u):
            g = u["g"]
            h, b, j = g["h"], g["b"], u["j"]
            hr, cw, A_, ak = u["hr"], u["cw"], u["A"], u["ak"]
            if u["first"]:
                g["y"], g["yk"] = pYr.next()
            y_, yk = g["y"], g["yk"]
            first, last = u["first"], u["last"]
            ph.op("pe", lambda e: e.matmul(y_[hr, cw], lhsT=Vt[:, j, h * 64:(h + 1) * 64], rhs=A_[:, cw],
                                           start=first, stop=last, skip_group_check=True), r=[ak] + vkeys, w=[yk])
            if last:
                o_, ok = ystr.next()
                q0 = b * QB
                ph.op("act", lambda e: e.copy(out=o_[hr, :], in_=y_[hr, :QB]), r=[yk], w=[ok])
                ph.dma(YA_d[h * 64:(h + 1) * 64, q0:q0 + QB], o_[hr, :], r=[ok])

        SKB, SKC = 3, 4
        n = len(units)
        for t in range(n + SKC):
            if t < n:
                stageA(units[t])
            if 0 <= t - SKB < n:
                stageB(units[t - SKB])
            if 0 <= t - SKC < n:
                stageC(units[t - SKC])

    if stop_after < 3:
        return nc

    with Phase(nc, "p3") as ph:
        cs = ph.sb("cs", [128, NCST], F32)
        identb = ph.sb("identb", [128, 128], BF16)
        OPb = ph.sb("OPb", [128, 128], BF16)
        wukf = ph.sb("wukf", [128, 4, 128], F32)
        wukb = ph.sb("wukb", [128, 4, 128], BF16)
        wuvf = ph.sb("wuvf", [128, 8, 64], F32)
        wuvb = ph.sb("wuvb", [128, 8, 64], BF16)
        CKVT = ph.sb("CKVT", [128, T], BF16)
        CKVt = ph.sb("CKVt", [128, NT, 128], BF16)
        KR = ph.sb("KR", [128, T], BF16)
        KI = ph.sb("KI", [128, T], BF16)
        QIb = [ph.sb(f"QIb{i}", [128, 4, QB], BF16) for i in range(2)]
        QBNb = [ph.sb(f"QBNb{i}", [128, 4, QB], BF16) for i in range(2)]
        QBRb = [ph.sb(f"QBRb{i}", [128, 2, QB], BF16) for i in range(2)]
        WIb = [ph.sb(f"WIb{i}", [128, 3, 8], F32) for i in range(2)]
        dg = [ph.sb(f"dg{i}", [128, 8, 128], BF16) for i in range(2)]
        Rr_ = [ph.sb(f"R{i}", [128, 512], BF16) for i in range(3)]
        score = [ph.sb(f"score{i}", [128, T], F32) for i in range(2)]
        junkb = ph.sb("junkb", [128, T], BF16)
        mb = [ph.sb(f"mb{i}", [128, 3, T], BF16) for i in range(2)]
        sm = [ph.sb(f"sm{i}", [128, 8 + 2 * (NITER + 2)], F32) for i in range(2)]
        steps = [ph.sb(f"steps{i}", [128, NITER + 1], F32) for i in range(2)]
        qlat = [ph.sb(f"qlat{i}", [128, QB], BF16) for i in range(2)]
        Pb = [ph.sb(f"P{i}", [128, QB], BF16) for i in range(3)]
        numS = [ph.sb(f"numS{i}", [128, QB], F32) for i in range(2)]
        denS = [ph.sb(f"denS{i}", [128, QB], F32) for i in range(2)]
        olat = [ph.sb(f"olat{i}", [128, QB], BF16) for i in range(2)]
        yst = [ph.sb(f"yst{i}", [128, QB], BF16) for i in range(2)]
        pI = [ph.ps(f"pI{i}", [128, 512]) for i in range(2)]
        pSc = [ph.ps("pSc0", [128, 512])]
        pLg = [ph.ps(f"pLg{i}", [128, 512]) for i in range(2)]
        pN = ph.ps("pN", [128, 512])
        pD = ph.ps("pD", [128, 512])
        pQY = ph.ps("pQY", [128, 512])

        ph.dma(cs[:, :], cst[:, :], w=["cs"])
        ph.op("pool", lambda e: e.tensor_copy(out=identb[:, :], in_=cs[:, C_ID:C_ID + 128]), r=["cs"], w=["identb"])
        ph.op("pool", lambda e: e.tensor_copy(out=OPb[:, :], in_=cs[:, C_OP:C_OP + 128]), r=["cs"], w=["OPb"])
        ph.dma(wukf[:, :, :], wukT[:, :, :], w=["wukf"])
        ph.op("pool", lambda e: e.tensor_copy(out=wukb[:, :, :], in_=wukf[:, :, :]), r=["wukf"], w=["wukb"])
        ph.dma(wuvf[:, :, :], wuv[:, :, :], w=["wuvf"])
        ph.op("pool", lambda e: e.tensor_copy(out=wuvb[:, :, :], in_=wuvf[:, :, :]), r=["wuvf"], w=["wuvb"])
        ph.dma(CKVT[:, :], CKVT_d[:, :], w=["CKVT"])
        ph.dma(KR[:, :], KR_d[:, :], w=["KR"])
        ph.dma(KI[:, :], KI_d[:, :], w=["KI"])
        for g in range(3):
            ph.dma(CKVt[:, g * 11:(g + 1) * 11, :],
                   CKV_d[g * 11 * 128:(g + 1) * 11 * 128, :].rearrange("(i p) n -> p i n", p=128), w=[("CKVt", g)])
        ckeys = [("CKVt", g) for g in range(3)]
        pIr, pLgr, Rr, Pr = Ring("pI", pI), Ring("pLg", pLg), Ring("R", Rr_), Ring("P", Pb)
        qlr, numr, denr, olr, ystr = Ring("qlat", qlat), Ring("numS", numS), Ring("denS", denS), Ring("olat", olat), Ring("yst", yst)
        scr, dgr, smr, stpr = Ring("score", score), Ring("dg", dg), Ring("sm", sm), Ring("steps", steps)

        def load_block(b):
            q0 = b * QB
            i2 = b % 2
            ph.dma(QIb[i2][:, :, :], QI_d[:, q0:q0 + QB].rearrange("(c p) q -> p c q", p=128), w=[("QIb", i2)])
            ph.dma(QBNb[i2][:, :, :], QBN_d[:, q0:q0 + QB].rearrange("(c p) q -> p c q", p=128), w=[("QBNb", i2)])
            ph.dma(QBRb[i2][:, :, :], QBR_d[:, q0:q0 + QB].rearrange("(c p) q -> p c q", p=128), w=[("QBRb", i2)])
            ph.dma(WIb[i2][:, :, :], WI_d[q0:q0 + QB, :].rearrange("(s p) h -> p s h", p=128), w=[("WIb", i2)])

        def gen_index(b):
            i2 = b % 2
            mb_, mbk = mb[i2], ("mb", i2)
            ph.op("pool", lambda e: e.memset(mb_[:, :, :], -30000.0), w=[mbk])
            yield

            def tile_gen(s):
                i = 3 * b + s
                nkt = min(i + 2, NT)
                Nk = nkt * 128
                sc, sck = scr.next()
                dg_, dgk = dgr.next()
                sm_, smk = smr.next()
                st_, stk = stpr.next()
                for h in range(8):
                    ph.op("act", lambda e, dg_=dg_, h=h, s=s: e.activation(
                        out=dg_[:, h, :], in_=identb[:, :], func=AF.Copy, scale=WIb[i2][:, s, h:h + 1]),
                        r=["identb", ("WIb", i2)], w=[dgk])
                a_, ak = pSc[0], ("pSc", 0)
                items = [(k0, min(512, Nk - k0), h) for k0 in range(0, Nk, 512) for h in range(8)]
                slot = {}

                def rec_I(n):
                    k0, w, h = items[n]
                    hr = slice((h % 2) * 64, (h % 2) * 64 + 64)
                    p_, pk = pIr.next()
                    slot[n] = (p_, pk)
                    ph.op("pe", lambda e: e.matmul(
                        p_[:, :w], lhsT=QIb[i2][hr, h // 2, s * 128:(s + 1) * 128], rhs=KI[hr, k0:k0 + w],
                        start=True, stop=True), r=[("QIb", i2), "KI"], w=[pk])

                def rec_RD(n):
                    k0, w, h = items[n]
                    p_, pk = slot.pop(n)
                    r_, rk = Rr.next()
                    ph.op("act", lambda e: e.activation(out=r_[:, :w], in_=p_[:, :w], func=AF.Relu), r=[pk], w=[rk])
                    ph.op("pe", lambda e: e.matmul(a_[:, :w], lhsT=dg_[:, h, :], rhs=r_[:, :w], start=(h == 0), stop=(h == 7)),
                          r=[dgk, rk], w=[ak])
                    if h == 7:
                        ph.op("act", lambda e: e.copy(out=sc[:, k0:k0 + w], in_=a_[:, :w]), r=[ak], w=[sck])

                rec_I(0)
                for n in range(len(items)):
                    if n + 1 < len(items):
                        rec_I(n + 1)
                    rec_RD(n)
                    if items[n][2] == 7:
                        yield
                ph.op("dve", lambda e, sc=sc, sm_=sm_, Nk=Nk: e.tensor_reduce(
                    out=sm_[:, 0:1], in_=sc[:, :Nk], axis=AX.X, op=ALU.max), r=[sck], w=[smk])
                ph.op("dve", lambda e, sc=sc, sm_=sm_, Nk=Nk: e.tensor_reduce(
                    out=sm_[:, 1:2], in_=sc[:, :Nk], axis=AX.X, op=ALU.min), r=[sck], w=[smk])
                ph.op("pool", lambda e, sc=sc, i=i: e.tensor_tensor(
                    out=sc[:, i * 128:(i + 1) * 128], in0=sc[:, i * 128:(i + 1) * 128], in1=cs[:, C_MD:C_MD + 128],
                    op=ALU.add), r=[sck, "cs"], w=[sck])
                if i + 1 < NT:
                    ph.op("pool", lambda e, sc=sc, i=i: e.tensor_tensor(
                        out=sc[:, (i + 1) * 128:(i + 2) * 128], in0=sc[:, (i + 1) * 128:(i + 2) * 128],
                        in1=cs[:, C_MN:C_MN + 128], op=ALU.add), r=[sck, "cs"], w=[sck])
                ph.op("dve", lambda e, sm_=sm_: e.tensor_tensor(out=sm_[:, 6:7], in0=sm_[:, 0:1], in1=sm_[:, 1:2],
                                                               op=ALU.subtract), r=[smk], w=[smk])
                ph.op("dve", lambda e, sm_=sm_: e.tensor_scalar(out=sm_[:, 7:8], in0=sm_[:, 6:7], scalar1=-1.0 / 64, scalar2=-1e-6,
                                                               op0=ALU.mult, op1=ALU.add), r=[smk], w=[smk])
                ph.op("dve", lambda e, sm_=sm_: e.tensor_tensor(out=sm_[:, 1:2], in0=sm_[:, 1:2], in1=sm_[:, 7:8],
                                                               op=ALU.add), r=[smk], w=[smk])
                ph.op("dve", lambda e, sm_=sm_: e.tensor_tensor(out=sm_[:, 2:3], in0=sm_[:, 0:1], in1=sm_[:, 1:2],
                                                               op=ALU.subtract), r=[smk], w=[smk])
                ph.op("dve", lambda e, sm_=sm_, st_=st_: e.tensor_scalar(
                    out=st_[:, :], in0=cs[:, C_P2:C_P2 + NITER + 1], scalar1=sm_[:, 2:3], scalar2=None, op0=ALU.mult),
                    r=[smk, "cs"], w=[stk])
                ph.op("dve", lambda e, sm_=sm_, st_=st_: e.tensor_tensor(out=sm_[:, 8:9], in0=sm_[:, 1:2], in1=st_[:, 0:1],
                                                                        op=ALU.add), r=[smk, stk], w=[smk])
                yield
                for it in range(NITER):
                    mid = sm_[:, 8 + it:9 + it]
                    mid2 = sm_[:, 9 + it:10 + it]
                    cnt = sm_[:, 3:4]
                    g_ = sm_[:, 4:5]
                    ph.op("dve", lambda e, sc=sc, mid=mid, cnt=cnt, Nk=Nk: e.tensor_scalar(
                        out=junkb[:, :Nk], in0=sc[:, :Nk], scalar1=mid, scalar2=None, op0=ALU.is_ge, op1=ALU.add,
                        accum_out=cnt), r=[sck, smk], w=[smk, "junkb"])
                    ph.op("dve", lambda e, cnt=cnt, g_=g_, st_=st_, it=it: e.tensor_scalar(
                        out=g_, in0=cnt, scalar1=TOPK - 0.5, scalar2=st_[:, it:it + 1], op0=ALU.is_ge, op1=ALU.mult),
                        r=[smk, stk], w=[smk])
                    ph.op("dve", lambda e, g_=g_, mid=mid, mid2=mid2, st_=st_, it=it: e.scalar_tensor_tensor(
                        out=mid2, in0=g_, scalar=st_[:, it + 1:it + 2], in1=mid, op0=ALU.subtract, op1=ALU.add),
                        r=[smk, stk], w=[smk])
                    yield
                lo = sm_[:, 5:6]
                ph.op("dve", lambda e, sm_=sm_, st_=st_, lo=lo: e.tensor_tensor(
                    out=lo, in0=sm_[:, 8 + NITER:9 + NITER], in1=st_[:, NITER:NITER + 1], op=ALU.subtract),
                    r=[smk, stk], w=[smk])
                ph.op("dve", lambda e, sc=sc, lo=lo, s=s, Nk=Nk: e.tensor_scalar(
                    out=mb_[:, s, :Nk], in0=sc[:, :Nk], scalar1=lo, scalar2=-30000.0, op0=ALU.is_lt, op1=ALU.mult),
                    r=[sck, smk], w=[mbk])
                yield

            for s in range(3):
                yield from tile_gen(s)

        def gen_attn(b):
            q0 = b * QB
            i2 = b % 2
            mb_, mbk = mb[i2], ("mb", i2)
            nkb = min(3 * b + 4, NT)
            pend = []

            def unit(h, j, ql, qlk):
                hr = slice((h % 2) * 64, (h % 2) * 64 + 64)
                g4 = h % 4
                r32 = slice(32 * g4, 32 * g4 + 32)
                lg, lgk = pLgr.next()
                P_, pk = Pr.next()
                ks = slice(j * 128, (j + 1) * 128)

                def lmm(e):
                    e.matmul(lg[:, :QB], lhsT=CKVT[:, ks], rhs=ql[:, :], start=True, stop=False)
                    e.matmul(lg[:, :QB], lhsT=KR[r32, ks], rhs=QBRb[i2][r32, h // 4, :], start=False, stop=False,
                             tile_position=(32 * g4, 0))
                    ins = None
                    for s in range(3):
                        ins = e.matmul(lg[:, s * 128:(s + 1) * 128], lhsT=mb_[:, s, ks], rhs=identb[:, :],
                                       start=False, stop=(s == 2))
                    return ins
                ph.op("pe", lmm, r=["CKVT", "KR", qlk, ("QBRb", i2), mbk, "identb"], w=[lgk])
                ph.op("act", lambda e: e.activation(out=P_[:, :], in_=lg[:, :QB], func=AF.Exp, scale=0.125), r=[lgk], w=[pk])

                def stage_b():
                    first, last = (j == 0), (j == nkb - 1)

                    def avmm(e):
                        e.matmul(pN[:, :QB], lhsT=CKVt[:, j, :], rhs=P_[:, :], start=first, stop=last)
                        return e.matmul(pD[:, :QB], lhsT=OPb[:, :], rhs=P_[:, :], start=first, stop=last)
                    ph.op("pe", avmm, r=[pk, "OPb"] + ckeys, w=["pN", "pD"])
                    if last:
                        nS, nk_ = numr.next()
                        dS, dk_ = denr.next()
                        ol, olk = olr.next()
                        ph.op("act", lambda e: e.copy(out=nS[:, :], in_=pN[:, :QB]), r=["pN"], w=[nk_])
                        ph.op("act", lambda e: e.copy(out=dS[:, :], in_=pD[:, :QB]), r=["pD"], w=[dk_])
                        ph.op("dve", lambda e: e.reciprocal(out=dS[:, :], in_=dS[:, :]), r=[dk_], w=[dk_])
                        ph.op("dve", lambda e: e.tensor_tensor(out=ol[:, :], in0=nS[:, :], in1=dS[:, :], op=ALU.mult),
                              r=[nk_, dk_], w=[olk])
                        ph.op("pe", lambda e: e.matmul(pQY[hr, :QB], lhsT=wuvb[:, h, :], rhs=ol[:, :], start=True, stop=True),
                              r=[olk, "wuvb"], w=["pQY"])
                        o_, ok = ystr.next()
                        ph.op("act", lambda e: e.copy(out=o_[hr, :], in_=pQY[hr, :QB]), r=["pQY"], w=[ok])
                        ph.dma(YB_d[h * 64:(h + 1) * 64, q0:q0 + QB], o_[hr, :], r=[ok])
                return stage_b

            def qlat_for(h):
                hr = slice((h % 2) * 64, (h % 2) * 64 + 64)
                ql, qlk = qlr.next()
                ph.op("pe", lambda e: e.matmul(pQY[:, :QB], lhsT=wukb[hr, h // 2, :], rhs=QBNb[i2][hr, h // 2, :],
                                               start=True, stop=True), r=["wukb", ("QBNb", i2)], w=["pQY"])
                ph.op("act", lambda e: e.copy(out=ql[:, :], in_=pQY[:, :QB]), r=["pQY"], w=[qlk])
                return ql, qlk

            for h in range(8):
                ql, qlk = qlat_for(h)
                for j in range(nkb):
                    sb_ = unit(h, j, ql, qlk)
                    if pend:
                        pend.pop(0)()
                    pend.append(sb_)
                    yield
            while pend:
                pend.pop(0)()
            yield

        def run_gens(gens):
            items = []
            for g, n in gens:
                items.append([g, n, 0])
            while items:
                items.sort(key=lambda t: t[2] / t[1])
                it = items[0]
                try:
                    next(it[0])
                    it[2] += 1
                except StopIteration:
                    items.remove(it)

        def n_index(b):
            n = 1
            for s in range(3):
                i = 3 * b + s
                Nk = min(i + 2, NT) * 128
                n += (Nk + 511) // 512 + 1 + NITER + 1
            return n

        load_block(0)
        run_gens([(gen_index(0), n_index(0))])
        for b in range(NQB):
            gens = [(gen_attn(b), 8 * (min(3 * b + 4, NT) + 1))]
            if b + 1 < NQB:
                load_block(b + 1)
                gens.append((gen_index(b + 1), n_index(b + 1)))
            run_gens(gens)


    if stop_after < 4:
        outer.close()
        return nc

    with Phase(nc, "p4") as ph:
        cs = ph.sb("cs", [128, NCST], F32)
        iocap = ph.sb("iocap", [128, 32], F32)
        g2B = ph.sb("g2B", [128, D], F32)
        brB = ph.sb("brB", [128, 36], F32)
        wrf = ph.sb("wrf", [128, 8, 36], F32)
        wgab = ph.sb("wgab", [128, 8, D], BF16)
        wgbb = ph.sb("wgbb", [128, 8, D], BF16)
        wuab = ph.sb("wuab", [128, 4, D], BF16)
        wubb = ph.sb("wubb", [128, 4, D], BF16)
        wob = ph.sb("wob", [128, 8, D], BF16)
        wst = [ph.sb(f"wst{i}", [128, 8, 512], F32) for i in range(2)]
        unb = [ph.sb(f"unb{i}", [128, 8, QB], BF16) for i in range(2)]
        yab = [ph.sb(f"yab{i}", [128, 4, QB], BF16) for i in range(2)]
        ybb = [ph.sb(f"ybb{i}", [128, 4, QB], BF16) for i in range(2)]
        sg = [ph.sb(f"sg{i}", [128, QB], F32) for i in range(4)]
        zab = [ph.sb(f"zab{i}", [128, QB], F32) for i in range(4)]
        zT = [ph.sb(f"zT{i}", [128, 8, QB], BF16) for i in range(2)]
        xt = [ph.sb(f"xt{i}", [128, D], F32) for i in range(2)]
        h1t = [ph.sb(f"h1t{i}", [128, D], F32) for i in range(2)]
        u2f = [ph.sb(f"u2f{i}", [128, D], F32) for i in range(2)]
        u2b = [ph.sb(f"u2b{i}", [128, D], BF16) for i in range(2)]
        u2T = [ph.sb(f"u2T{i}", [128, 8, 128], F32) for i in range(2)]
        junk = ph.sb("junk", [128, D], BF16)
        rs = [ph.sb(f"rs{i}", [128, 128], F32) for i in range(2)]
        ACCA = ph.sb("ACCA", [128, 32], F32)
        sli = [ph.sb(f"sli{i}", [128, 2], I32) for i in range(2)]
        lgs = [ph.sb(f"lgs{i}", [128, 36], F32) for i in range(2)]
        asum = [ph.sb(f"asum{i}", [128, 32], F32) for i in range(2)]
        cum = [ph.sb(f"cum{i}", [128, 96], F32) for i in range(2)]
        pr = [ph.ps(f"pr{i}", [128, 512]) for i in range(4)]
        pM = [ph.ps(f"pM{i}", [128, 512]) for i in range(2)]
        pTf = ph.ps("pTf", [128, 1024])

        ph.dma(cs[:, :], cst[:, :], w=["cs"])
        ph.dma(g2B[:, :], g2[0:1, :].partition_broadcast(128), w=["g2B"])
        ph.dma(brB[:, :], br[0:1, :].partition_broadcast(128), w=["brB"])
        ph.dma(wrf[:, :, :], wr[:, :].rearrange("(c p) n -> p c n", p=128), w=["wrf"])
        ph.op("pool", lambda e: e.tensor_scalar(out=iocap[:, :], in0=cs[:, C_IO:C_IO + 32], scalar1=float(CAP), scalar2=None,
                                                op0=ALU.mult), r=["cs"], w=["iocap"])
        ph.op("pool", lambda e: e.memset(ACCA[:, :], 0.0), w=["ACCA"])
        wsr = Ring("wst", wst)

        def load_w(src, nch, dest, dk):
            ncol = src.shape[1]
            for c0 in range(0, ncol, 512):
                ws, wk = wsr.next()
                ph.dma(ws[:, :nch, :], src[:, c0:c0 + 512].rearrange("(c p) n -> p c n", p=128), w=[wk])
                ph.op("act", lambda e, ws=ws, c0=c0: e.copy(out=dest[:, :, c0:c0 + 512], in_=ws[:, :nch, :]),
                      r=[wk], w=[dk])
        load_w(winB[:, B_GA:B_GA + D], 8, wgab, "wgab")
        load_w(winB[:, B_GB:B_GB + D], 8, wgbb, "wgbb")
        load_w(wupa, 4, wuab, "wuab")
        load_w(wupb, 4, wubb, "wubb")
        load_w(wo, 8, wob, "wob")
        prr, sgr, zabr = Ring("pr", pr), Ring("sg", sg), Ring("zab", zab)

        def mmn(e, ps_ap, n, lhs_fn, rhs_fn):
            ins = None
            for c in range(n):
                ins = e.matmul(ps_ap, lhsT=lhs_fn(c), rhs=rhs_fn(c), start=(c == 0), stop=(c == n - 1))
            return ins

        def do_block(b):
            q0 = b * QB
            i2 = b % 2
            ph.dma(unb[i2][:, :, :], unT_d[:, :, q0:q0 + QB], w=[("unb", i2)])
            ph.dma(yab[i2][:, :, :], YA_d[:, q0:q0 + QB].rearrange("(c p) q -> p c q", p=128), w=[("yab", i2)])
            ph.dma(ybb[i2][:, :, :], YB_d[:, q0:q0 + QB].rearrange("(c p) q -> p c q", p=128), w=[("ybb", i2)])
            zk = ("zT", i2)
            for m in range(8):
                ms = slice(m * 128, (m + 1) * 128)
                parts = []
                for (wg, wgk, wu, wuk, yb_, ybk) in ((wgab, "wgab", wuab, "wuab", yab[i2], ("yab", i2)),
                                                     (wgbb, "wgbb", wubb, "wubb", ybb[i2], ("ybb", i2))):
                    pg, pgk = prr.next()
                    pu, puk = prr.next()
                    s_, sk = sgr.next()
                    z_, zk2 = zabr.next()
                    ph.op("pe", lambda e, pg=pg, wg=wg, ms=ms: mmn(e, pg[:, :QB], 8, lambda c: wg[:, c, ms],
                                                                  lambda c: unb[i2][:, c, :]),
                          r=[wgk, ("unb", i2)], w=[pgk])
                    ph.op("act", lambda e, pg=pg, s_=s_: e.activation(out=s_[:, :], in_=pg[:, :QB], func=AF.Sigmoid),
                          r=[pgk], w=[sk])
                    ph.op("pe", lambda e, pu=pu, wu=wu, ms=ms, yb_=yb_: mmn(e, pu[:, :QB], 4, lambda c: wu[:, c, ms],
                                                                          lambda c: yb_[:, c, :]),
                          r=[wuk, ybk], w=[puk])
                    ph.op("dve", lambda e, pu=pu, s_=s_, z_=z_: e.tensor_tensor(out=z_[:, :], in0=pu[:, :QB], in1=s_[:, :],
                                                                               op=ALU.mult), r=[puk, sk], w=[zk2])
                    parts.append((z_, zk2))
                ph.op("pool", lambda e, m=m, parts=parts: e.tensor_tensor(
                    out=zT[i2][:, m, :], in0=parts[0][0][:, :], in1=parts[1][0][:, :], op=ALU.add),
                    r=[parts[0][1], parts[1][1]], w=[zk])
            def do_tile(s):
                i = 3 * b + s
                j2 = i % 2
                ts = slice(s * 128, (s + 1) * 128)
                ph.dma(xt[j2][:, :], h0[i * 128:(i + 1) * 128, :], w=[("xt", j2)])
                for half in range(2):
                    ph.op("pe", lambda e, half=half, ts=ts: mmn(
                        e, pM[half][:, :], 8, lambda c: zT[i2][:, c, ts], lambda c: wob[:, c, half * 512:(half + 1) * 512]),
                        r=[zk, "wob"], w=[("pM", half)])
                    ph.op("dve", lambda e, half=half, j2=j2: e.tensor_tensor(
                        out=h1t[j2][:, half * 512:(half + 1) * 512], in0=pM[half][:, :],
                        in1=xt[j2][:, half * 512:(half + 1) * 512], op=ALU.add),
                        r=[("pM", half), ("xt", j2)], w=[("h1t", j2, half)])
                hk = [("h1t", j2, 0), ("h1t", j2, 1)]
                ph.dma(H1_d[i * 128:(i + 1) * 128, :], h1t[j2][:, :], r=hk)
                R = rs[j2]
                rk = ("rs", j2)
                ph.op("act", lambda e, j2=j2, R=R: e.activation(out=junk[:, :], in_=h1t[j2][:, :], func=AF.Square,
                                                               accum_out=R[:, 0:1]), r=hk, w=["junk", rk])
                ph.op("act", lambda e, R=R: e.activation(out=R[:, 1:2], in_=R[:, 0:1], func=AF.Sqrt, bias=EPS, scale=1.0 / D),
                      r=[rk], w=[rk])
                ph.op("dve", lambda e, R=R: e.reciprocal(out=R[:, 2:3], in_=R[:, 1:2]), r=[rk], w=[rk])
                ph.op("dve", lambda e, j2=j2, R=R: e.scalar_tensor_tensor(
                    out=u2f[j2][:, :], in0=h1t[j2][:, :], scalar=R[:, 2:3], in1=g2B[:, :], op0=ALU.mult, op1=ALU.mult),
                    r=hk + [rk, "g2B"], w=[("u2f", j2)])
                ph.op("pool", lambda e, j2=j2: e.tensor_copy(out=u2b[j2][:, :], in_=u2f[j2][:, :]),
                      r=[("u2f", j2)], w=[("u2b", j2)])

                def trf(e, j2=j2):
                    ins = None
                    for c in range(8):
                        ins = e.transpose(out=pTf[:, c * 128:(c + 1) * 128], in_=u2f[j2][:, c * 128:(c + 1) * 128],
                                          identity=cs[:, C_ID:C_ID + 128])
                    return ins
                ph.op("pe", trf, r=[("u2f", j2), "cs"], w=["pTf"])
                ph.op("act", lambda e, j2=j2: e.copy(out=u2T[j2][:, :, :], in_=pTf[:, :].rearrange("p (c t) -> p c t", c=8)),
                      r=["pTf"], w=[("u2T", j2)])
                pq, pqk = prr.next()
                ph.op("pe", lambda e, pq=pq, j2=j2: mmn(e, pq[:, :36], 8, lambda c: u2T[j2][:, c, :], lambda c: wrf[:, c, :]),
                      r=[("u2T", j2), "wrf"], w=[pqk])
                V = lambda a, b2: R[:, a:b2]
                lgk = ("lg", j2)
                L = lgs[j2]
                ph.op("dve", lambda e, pq=pq, L=L: e.tensor_tensor(out=L[:, :], in0=pq[:, :36], in1=brB[:, :], op=ALU.add),
                      r=[pqk, "brB"], w=[lgk])
                d = lambda fn, r_=(), w_=(): ph.op("dve", fn, r=[rk, lgk] + list(r_), w=[rk] + list(w_))
                d(lambda e, L=L, R=R: e.tensor_reduce(out=R[:, 3:4], in_=L[:, 0:4], axis=AX.X, op=ALU.max))
                d(lambda e, L=L, R=R: e.tensor_scalar(out=R[:, 16:20], in0=L[:, 0:4], scalar1=R[:, 3:4], scalar2=None,
                                                      op0=ALU.is_equal))
                d(lambda e, R=R: e.tensor_scalar(out=R[:, 4:5], in0=R[:, 3:4], scalar1=-1.0, scalar2=None, op0=ALU.mult))
                ph.op("act", lambda e, L=L, R=R: e.activation(out=R[:, 24:28], in_=L[:, 0:4], func=AF.Exp, bias=R[:, 4:5],
                                                             accum_out=R[:, 5:6]), r=[rk, lgk], w=[rk])
                d(lambda e, R=R: e.tensor_scalar(out=R[:, 20:24], in0=R[:, 16:20], scalar1=-1.0, scalar2=1e9,
                                                 op0=ALU.add, op1=ALU.mult))
                for g in range(4):
                    d(lambda e, L=L, R=R, g=g: e.tensor_scalar(
                        out=R[:, 32 + g * 8:40 + g * 8], in0=L[:, 4 + g * 8:12 + g * 8], scalar1=R[:, 20 + g:21 + g],
                        scalar2=None, op0=ALU.add))
                d(lambda e, R=R: e.tensor_reduce(out=R[:, 6:7], in_=R[:, 32:64], axis=AX.X, op=ALU.max))
                d(lambda e, R=R: e.tensor_scalar(out=R[:, 64:96], in0=R[:, 32:64], scalar1=R[:, 6:7], scalar2=None,
                                                 op0=ALU.is_equal))
                d(lambda e, R=R: e.scalar_tensor_tensor(out=R[:, 32:64], in0=R[:, 64:96], scalar=-1e9, in1=R[:, 32:64],
                                                        op0=ALU.mult, op1=ALU.add))
                d(lambda e, R=R: e.tensor_reduce(out=R[:, 7:8], in_=R[:, 32:64], axis=AX.X, op=ALU.max))
                d(lambda e, R=R: e.tensor_scalar(out=R[:, 96:128], in0=R[:, 32:64], scalar1=R[:, 7:8], scalar2=None,
                                                 op0=ALU.is_equal))
                d(lambda e, R=R: e.tensor_tensor(out=R[:, 8:9], in0=R[:, 7:8], in1=R[:, 6:7], op=ALU.subtract))
                ph.op("act", lambda e, R=R: e.activation(out=R[:, 9:10], in_=R[:, 8:9], func=AF.Exp), r=[rk], w=[rk])
                d(lambda e, R=R: e.tensor_scalar(out=R[:, 10:11], in0=R[:, 9:10], scalar1=1.0, scalar2=R[:, 5:6],
                                                 op0=ALU.add, op1=ALU.mult))
                d(lambda e, R=R: e.reciprocal(out=R[:, 11:12], in_=R[:, 10:11]))
                d(lambda e, R=R: e.tensor_tensor(out=R[:, 12:13], in0=R[:, 11:12], in1=R[:, 9:10], op=ALU.mult))
                d(lambda e, R=R, i=i: e.tensor_copy(out=gates_all[:, i, :], in_=R[:, 11:13]), w_=["gates"])
                A_, Ak = asum[j2], ("asum", j2)
                d(lambda e, R=R, A_=A_: e.tensor_tensor(out=A_[:, :], in0=R[:, 64:96], in1=R[:, 96:128], op=ALU.add), w_=[Ak])
                pc, pck = prr.next()

                def cmm(e, pc=pc, A_=A_):
                    e.matmul(pc[:, :32], lhsT=cs[:, C_TS:C_TS + 128], rhs=A_[:, :], start=True, stop=False)
                    return e.matmul(pc[:, :32], lhsT=cs[:, C_OP:C_OP + 128], rhs=ACCA[:, :], start=False, stop=True)
                ph.op("pe", cmm, r=[Ak, "ACCA", "cs"], w=[pck])
                ph.op("pool", lambda e, A_=A_: e.tensor_tensor(out=ACCA[:, :], in0=ACCA[:, :], in1=A_[:, :], op=ALU.add),
                      r=[Ak, "ACCA"], w=["ACCA"])
                C_, Ck = cum[j2], ("cum", j2)
                ph.op("dve", lambda e, pc=pc, C_=C_: e.tensor_tensor(out=C_[:, 0:32], in0=pc[:, :32], in1=iocap[:, :],
                                                                    op=ALU.add), r=[pck, "iocap"], w=[Ck])
                for k, oc in ((0, 64), (1, 96)):
                    d(lambda e, C_=C_, R=R, oc=oc, k=k: e.tensor_tensor(out=C_[:, 32 + 32 * k:64 + 32 * k], in0=C_[:, 0:32],
                                                                       in1=R[:, oc:oc + 32], op=ALU.mult), r_=[Ck], w_=[Ck])
                    d(lambda e, C_=C_, R=R, k=k: e.tensor_reduce(out=R[:, 13 + k:14 + k], in_=C_[:, 32 + 32 * k:64 + 32 * k],
                                                                axis=AX.X, op=ALU.add), r_=[Ck])
                slk = ("sli", j2)
                d(lambda e, R=R, j2=j2: e.tensor_scalar(out=sli[j2][:, :], in0=R[:, 13:15], scalar1=0.25, scalar2=None,
                                                       op0=ALU.add), w_=[slk])
                ph.op("pool", lambda e, j2=j2, i=i: e.tensor_copy(out=slots_all[:, i, :], in_=sli[j2][:, :]),
                      r=[slk], w=["slots"])
                for k in range(2):
                    ph.op("pool", lambda e, j2=j2, k=k: e.indirect_dma_start(
                        out=XS_d[:, :], out_offset=bass.IndirectOffsetOnAxis(ap=sli[j2][:, k:k + 1], axis=0),
                        in_=u2b[j2][:, :], in_offset=None),
                        r=[slk, ("u2b", j2)], w=[], dma=True)
            for s in range(3):
                do_tile(s)
        for b in range(NQB):
            do_block(b)

    if stop_after < 5:
        outer.close()
        return nc

    with Phase(nc, "p5") as ph:
        cs = ph.sb("cs", [128, 128], F32)
        identb = ph.sb("identb", [128, 128], BF16)
        wst = [ph.sb(f"wst{i}", [128, 8, 512], F32) for i in range(6)]
        w1b = [ph.sb(f"w1b{i}", [128, 8, 512], BF16) for i in range(2)]
        w3b = [ph.sb(f"w3b{i}", [128, 8, 512], BF16) for i in range(2)]
        w2b = [ph.sb(f"w2b{i}", [128, 4, D], BF16) for i in range(2)]
        xs = [ph.sb(f"xs{i}", [128, 3, D], BF16) for i in range(2)]
        xsT = [ph.sb(f"xsT{i}", [128, 8, CAP], BF16) for i in range(2)]
        sil = [ph.sb(f"sil{i}", [128, CAP], F32) for i in range(2)]
        hid = [ph.sb(f"hid{i}", [128, 4, CAP], BF16) for i in range(2)]
        yt = [ph.sb(f"yt{i}", [128, D], F32) for i in range(2)]
        pT = [ph.ps(f"pT{i}", [128, 1024], BF16) for i in range(2)]
        pH = [ph.ps(f"pH{i}", [128, 512]) for i in range(4)]
        pY = [ph.ps(f"pY{i}", [128, 512]) for i in range(2)]
        ph.dma(cs[:, :], cst[:, C_ID:C_ID + 128], w=["cs"])
        ph.op("pool", lambda e: e.tensor_copy(out=identb[:, :], in_=cs[:, :]), r=["cs"], w=["identb"])
        wsr, pTr, pHr, pYr, silr, ytr = Ring("wst", wst), Ring("pT", pT), Ring("pH", pH), Ring("pY", pY), Ring("sil", sil), Ring("yt", yt)

        def mmn(e, ps_ap, n, lhs_fn, rhs_fn):
            ins = None
            for c in range(n):
                ins = e.matmul(ps_ap, lhsT=lhs_fn(c), rhs=rhs_fn(c), start=(c == 0), stop=(c == n - 1))
            return ins

        def part1(ex):
            i2 = ex % 2
            ws, wk = wsr.next()
            ph.dma(ws[:, :, :], w1[ex].rearrange("(c p) n -> p c n", p=128), w=[wk])
            ph.op("act", lambda e, ws=ws, i2=i2: e.copy(out=w1b[i2][:, :, :], in_=ws[:, :, :]), r=[wk], w=[("w1b", i2)])
            ws, wk = wsr.next()
            ph.dma(ws[:, :, :], w3[ex].rearrange("(c p) n -> p c n", p=128), w=[wk], eng="act")
            ph.op("act", lambda e, ws=ws, i2=i2: e.copy(out=w3b[i2][:, :, :], in_=ws[:, :, :]), r=[wk], w=[("w3b", i2)])
            ws, wk = wsr.next()
            wsv = ws[:, :, :].rearrange("p c n -> p (c n)").rearrange("p (c n) -> p c n", c=4)
            ph.dma(wsv, w2[ex].rearrange("(c p) n -> p c n", p=128), w=[wk], eng="act")
            ph.op("dve", lambda e, wsv=wsv, i2=i2: e.tensor_copy(out=w2b[i2][:, :, :], in_=wsv), r=[wk], w=[("w2b", i2)])
            ph.dma(xs[i2][:, :, :], XS_d[ex * CAP:(ex + 1) * CAP, :].rearrange("(s p) d -> p s d", p=128), w=[("xs", i2)])
            for s in range(3):
                p_, pk = pTr.next()

                def tr(e, p_=p_, s=s, i2=i2):
                    ins = None
                    for c in range(8):
                        ins = e.transpose(out=p_[:, c * 128:(c + 1) * 128], in_=xs[i2][:, s, c * 128:(c + 1) * 128],
                                          identity=identb[:, :])
                    return ins
                ph.op("pe", tr, r=[("xs", i2), "identb"], w=[pk])
                ph.op("act", lambda e, p_=p_, s=s, i2=i2: e.copy(
                    out=xsT[i2][:, :, s * 128:(s + 1) * 128], in_=p_[:, :].rearrange("p (c t) -> p c t", c=8)),
                    r=[pk], w=[("xsT", i2, s)])
            xk = [("xsT", i2, s) for s in range(3)]
            for f in range(4):
                fs = slice(f * 128, (f + 1) * 128)
                pa, pak = pHr.next()
                pb, pbk = pHr.next()
                sl, slk = silr.next()
                ph.op("pe", lambda e, pa=pa, fs=fs, i2=i2: mmn(e, pa[:, :CAP], 8, lambda c: w1b[i2][:, c, fs],
                                                              lambda c: xsT[i2][:, c, :]), r=[("w1b", i2)] + xk, w=[pak])
                ph.op("pe", lambda e, pb=pb, fs=fs, i2=i2: mmn(e, pb[:, :CAP], 8, lambda c: w3b[i2][:, c, fs],
                                                              lambda c: xsT[i2][:, c, :]), r=[("w3b", i2)] + xk, w=[pbk])
                ph.op("act", lambda e, pa=pa, sl=sl: e.activation(out=sl[:, :], in_=pa[:, :CAP], func=AF.Silu), r=[pak], w=[slk])
                ph.op("dve", lambda e, pb=pb, sl=sl, f=f, i2=i2: e.tensor_tensor(
                    out=hid[i2][:, f, :], in0=pb[:, :CAP], in1=sl[:, :], op=ALU.mult), r=[pbk, slk], w=[("hid", i2, f)])

        def part2(ex):
            i2 = ex % 2
            hk = [("hid", i2, f) for f in range(4)]
            for s in range(3):
                y_, yk = ytr.next()
                for half in range(2):
                    py, pyk = pYr.next()
                    ph.op("pe", lambda e, py=py, s=s, half=half, i2=i2: mmn(
                        e, py[:, :], 4, lambda c: hid[i2][:, c, s * 128:(s + 1) * 128],
                        lambda c: w2b[i2][:, c, half * 512:(half + 1) * 512]), r=hk + [("w2b", i2)], w=[pyk])
                    ph.op("act", lambda e, py=py, y_=y_, half=half: e.copy(out=y_[:, half * 512:(half + 1) * 512], in_=py[:, :]),
                          r=[pyk], w=[yk + (half,)])
                ph.dma(YS_d[ex * CAP + s * 128:ex * CAP + (s + 1) * 128, :], y_[:, :], r=[yk + (0,), yk + (1,)])

        for ex in range(NEXP + 1):
            if ex < NEXP:
                part1(ex)
            if ex >= 1:
                part2(ex - 1)

    with Phase(nc, "p6") as ph:
        gFB = ph.sb("gFB", [128, D], F32)
        y1 = [ph.sb(f"y1_{i}", [128, D], F32) for i in range(4)]
        y2 = [ph.sb(f"y2_{i}", [128, D], F32) for i in range(4)]
        h1t = [ph.sb(f"h1t{i}", [128, D], F32) for i in range(4)]
        acc = [ph.sb(f"acc{i}", [128, D], F32) for i in range(4)]
        ot = [ph.sb(f"ot{i}", [128, D], F32) for i in range(4)]
        junk = ph.sb("junk", [128, D], BF16)
        st = ph.sb("st", [128, NT, 4], F32)
        ph.dma(gFB[:, :], gF[0:1, :].partition_broadcast(128), w=["gFB"])
        for i in range(NT):
            j2 = i % 4
            for k, yy in ((0, y1), (1, y2)):
                ph.op("pool", lambda e, yy=yy, j2=j2, i=i, k=k: e.indirect_dma_start(
                    out=yy[j2][:, :], out_offset=None, in_=YS_d[:, :],
                    in_offset=bass.IndirectOffsetOnAxis(ap=slots_all[:, i, k:k + 1], axis=0)),
                    r=[], w=[("y", k, j2)], dma=True)
            ph.dma(h1t[j2][:, :], H1_d[i * 128:(i + 1) * 128, :], w=[("h1t", j2)])
            ph.op("dve", lambda e, j2=j2, i=i: e.scalar_tensor_tensor(
                out=acc[j2][:, :], in0=y1[j2][:, :], scalar=gates_all[:, i, 0:1], in1=h1t[j2][:, :], op0=ALU.mult, op1=ALU.add),
                r=[("y", 0, j2), ("h1t", j2)], w=[("acc", j2)])
            ph.op("dve", lambda e, j2=j2, i=i: e.scalar_tensor_tensor(
                out=acc[j2][:, :], in0=y2[j2][:, :], scalar=gates_all[:, i, 1:2], in1=acc[j2][:, :], op0=ALU.mult, op1=ALU.add),
                r=[("y", 1, j2), ("acc", j2)], w=[("acc", j2)])
            ph.op("act", lambda e, j2=j2, i=i: e.activation(out=junk[:, :], in_=acc[j2][:, :], func=AF.Square,
                                                           accum_out=st[:, i, 0:1]), r=[("acc", j2)], w=["junk", ("st", i)])
            ph.op("act", lambda e, i=i: e.activation(out=st[:, i, 1:2], in_=st[:, i, 0:1], func=AF.Sqrt, bias=EPS, scale=1.0 / D),
                  r=[("st", i)], w=[("st", i)])
            ph.op("dve", lambda e, i=i: e.reciprocal(out=st[:, i, 2:3], in_=st[:, i, 1:2]), r=[("st", i)], w=[("st", i)])
            ph.op("dve", lambda e, j2=j2, i=i: e.scalar_tensor_tensor(
                out=ot[j2][:, :], in0=acc[j2][:, :], scalar=st[:, i, 2:3], in1=gFB[:, :], op0=ALU.mult, op1=ALU.mult),
                r=[("acc", j2), ("st", i), "gFB"], w=[("ot", j2)])
            lo_t = max(i * 128, NMETA)
            hi_t = min((i + 1) * 128, NMETA + SEQ)
            ph.dma(out[lo_t - NMETA:hi_t - NMETA, :], ot[j2][lo_t - i * 128:hi_t - i * 128, :], r=[("ot", j2)])

    outer.close()
    return nc


def _sel_cols(w, idx):
    idx = np.asarray(idx)
    o = w[:, np.maximum(idx, 0)].copy()
    o[:, idx < 0] = 0.0
    return o


def _winB_index():
    idx = -np.ones(NB, dtype=np.int64)
    for p in range(4):
        for hh in range(2):
            h = 2 * p + hh
            for n in range(48):
                idx[B_QBN + p * 128 + hh * 64 + n] = O_QB + h * 64 + 16 + n
    for h in range(8):
        for r in range(16):
            idx[B_QBR + h * 32 + r] = O_QB + h * 64 + r
            idx[B_QBRT + h * 32 + r] = O_QB + h * 64 + (r + 8) % 16
    for g in range(4):
        for r in range(16):
            idx[B_KR + g * 32 + r] = O_KR + r
            idx[B_KRT + g * 32 + r] = O_KR + (r + 8) % 16
    for g in range(2):
        for d in range(64):
            idx[B_KI + g * 64 + d] = O_KI + d
            if d < 16:
                idx[B_KIT + g * 64 + d] = O_KI + (d + 8) % 16
    for half, (bm, bt) in enumerate(((B_QI0, B_QI0T), (B_QI1, B_QI1T))):
        for j in range(256):
            col = half * 256 + j
            h, d = col // 64, col % 64
            idx[bm + j] = O_QI + col
            if d < 16:
                idx[bt + j] = O_QI + h * 64 + (d + 8) % 16
    for c in range(128):
        idx[B_CKV + c] = O_CKV + c
    for h in range(8):
        idx[B_WI + h] = O_WI + h
    for j in range(1024):
        idx[B_GA + j] = O_GA + j
        idx[B_GB + j] = O_GB + j
    return idx


def _tables():
    half = 8
    inv = (np.float32(500000.0) ** (-np.arange(half, dtype=np.float32) / np.float32(half))).astype(np.float32)
    pos = np.arange(T, dtype=np.float32)
    ang = (pos[:, None] * inv[None, :]).astype(np.float32)
    cos = np.cos(ang).astype(np.float32).T
    sin = np.sin(ang).astype(np.float32).T
    tabs = np.zeros((4, 128, T), np.float32)
    for ts, period in ((0, 32), (1, 64)):
        for r in range(128):
            rr = r % period
            if rr < 16:
                j = rr % 8
                tabs[2 * ts, r] = cos[j]
                tabs[2 * ts + 1, r] = (-sin[j]) if rr < 8 else sin[j]
            else:
                tabs[2 * ts, r] = 1.0
    return tabs


def _consts():
    c = np.zeros((128, NCST), np.float32)
    k = np.arange(128)
    c[:, C_ID:C_ID + 128] = np.eye(128, dtype=np.float32)
    c[:, C_TN:C_TN + 128] = -(k[:, None] >= k[None, :]).astype(np.float32)
    c[:, C_ON:C_ON + 128] = -1.0
    c[:, C_OP:C_OP + 128] = 1.0
    c[:, C_TS:C_TS + 128] = (k[:, None] < k[None, :]).astype(np.float32)
    q = np.arange(QB)
    for m in range(3):
        c[:, C_SBM + m * QB:C_SBM + (m + 1) * QB] = ((128 * m + k)[:, None] < q[None, :]).astype(np.float32)
    g = np.where(k < 16, 0, np.where(k < 80, 1, 2))
    NEG = -1e30
    c[:, C_MD:C_MD + 128] = np.where(g[None, :] <= g[:, None], 0.0, NEG)
    c[:, C_MN:C_MN + 128] = np.where((k[None, :] < 16) & (k[:, None] >= 80), 0.0, NEG)
    c[:, C_IO:C_IO + 32] = np.arange(32, dtype=np.float32)[None, :]
    for i in range(NITER + 1):
        c[:, C_P2 + i] = 2.0 ** (-(i + 1))
    return c


_CACHE = {}


def _host_inputs(inputs):
    f = lambda a: np.ascontiguousarray(np.asarray(a, dtype=np.float32))
    w_in = f(inputs["w_in"])[0]
    shared = {
        "g1": f(inputs["norm_mix_g"])[0:1],
        "g2": f(inputs["norm_ffn_g"])[0:1],
        "gF": f(inputs["norm_final_g"]).reshape(1, D),
        "winA": np.ascontiguousarray(w_in[:, 0:1536]),
        "winB": np.ascontiguousarray(_sel_cols(w_in, _winB_index())),
        "tabs": _tables(),
        "cst": _consts(),
        "wupa": f(inputs["w_up_a"])[0],
        "wupb": f(inputs["w_up_b"])[0],
        "wo": f(inputs["w_o"])[0],
        "wr": np.ascontiguousarray(np.concatenate([f(inputs["w_group"])[0], f(inputs["w_router"])[0]], axis=1)),
        "br": np.ascontiguousarray(np.concatenate([f(inputs["b_group"])[0], f(inputs["b_router"])[0]])[None, :]),
        "w1": f(inputs["w1"])[0],
        "w3": f(inputs["w3"])[0],
        "w2": f(inputs["w2"])[0],
    }
    wuk = f(inputs["w_uk"])[0]
    wukT = np.zeros((128, 4, 128), np.float32)
    for h in range(8):
        wukT[(h % 2) * 64:(h % 2) * 64 + 48, h // 2, :] = wuk[h].T
    shared["wukT"] = wukT
    shared["wuv"] = np.ascontiguousarray(np.transpose(f(inputs["w_uv"])[0], (1, 0, 2)))
    x = f(inputs["x"])
    meta = f(inputs["meta_tokens"])
    maps = []
    for b in range(NCORES):
        h0 = np.zeros((T, D), np.float32)
        h0[:NMETA] = meta
        h0[NMETA:NMETA + SEQ] = x[b]
        m = dict(shared)
        m["h0"] = h0
        maps.append(m)
    return maps


def kernel(**inputs):
    if "nc" not in _CACHE:
        _CACHE["nc"] = build_program()
    nc = _CACHE["nc"]
    maps = _host_inputs(inputs)
    res = run_bass_kernel_spmd(nc, maps, core_ids=list(range(NCORES)))
    return np.stack([np.asarray(r["out"], dtype=np.float32) for r in res.results], axis=0)
```
